# Optimizing a Trainium2 kernel written in Bass

```python
import jax
import jax.numpy as jnp
from jax import lax
import numpy as np

D_MODEL = 2048
BATCH = 4
SEQ = 2048
DEPTH = 4

GRID_W = 64
CTX_LEN = 256
N_MIXERS = 2
N_LRU_LAYERS = (DEPTH + N_MIXERS - 1) // N_MIXERS
N_ATTN_LAYERS = DEPTH // N_MIXERS
DN_ALPHA = (2.0 * DEPTH) ** 0.25
DN_BETA = (8.0 * DEPTH) ** -0.25
LN_EPS = 1e-5

LRU_WIDTH = D_MODEL
LRU_BLOCK_W = 256
LRU_BLOCKS = LRU_WIDTH // LRU_BLOCK_W
CONV_W = 4
CONV_LEFT = 1
LRU_C = 8.0

HEAD_DIM = 64
N_HEADS = D_MODEL // HEAD_DIM
N_KV_HEADS = N_HEADS // 8
GROUP = N_HEADS // N_KV_HEADS
Q_DIM = N_HEADS * HEAD_DIM
KV_DIM = N_KV_HEADS * HEAD_DIM
WINDOW = 128
Q_BLOCK = WINDOW
ROPE_BASE = 10000.0
NEG_INF = -1e30

N_EXPERTS = 32
TOP_K = 4
D_EXPERT = 768
SWIGLU_LIMIT = 7.0
SWIGLU_ALPHA = 1.702
EXPERT_BLOCK = 128

kernel_name = 'hybrid_rglru_swa_moe_diffusion_trunk'


def layer_norm(x, g, b):
    xf = x.astype(jnp.float32)
    mu = jnp.mean(xf, axis=-1, keepdims=True)
    var = jnp.mean(jnp.square(xf - mu), axis=-1, keepdims=True)
    return ((xf - mu) * lax.rsqrt(var + LN_EPS) * g + b).astype(x.dtype)


def rope_axis(x, pos):
    n = x.shape[-1]
    freqs = ROPE_BASE ** (-jnp.arange(0, n, 2, dtype=jnp.float32) / n)
    ang = pos[:, None] * freqs[None, :]
    cos = jnp.cos(ang)[None, :, None, :].astype(x.dtype)
    sin = jnp.sin(ang)[None, :, None, :].astype(x.dtype)
    x1, x2 = x[..., : n // 2], x[..., n // 2:]
    return jnp.concatenate([x1 * cos - x2 * sin, x1 * sin + x2 * cos], axis=-1)


def rope_2d(x, row_pos, col_pos):
    half = x.shape[-1] // 2
    return jnp.concatenate([rope_axis(x[..., :half], row_pos),
                            rope_axis(x[..., half:], col_pos)], axis=-1)


def centred_depthwise_conv(u, w, b):
    L = u.shape[1]
    up = jnp.pad(u, ((0, 0), (CONV_LEFT, CONV_W - 1 - CONV_LEFT), (0, 0)))
    out = b
    for k in range(CONV_W):
        out = out + up[:, k:k + L] * w[k]
    return out


def lru_coefficients(u, gate_w, gate_b, lam):
    B, L, _ = u.shape
    uf = u.astype(jnp.float32)
    ub = uf.reshape(B, L, LRU_BLOCKS, LRU_BLOCK_W)
    g = jnp.einsum('blnk,gnkj->gblnj', ub, gate_w.astype(jnp.float32)).reshape(2, B, L, LRU_WIDTH)
    g = g + gate_b.astype(jnp.float32)[:, None, None, :]
    r = jax.nn.sigmoid(g[0])
    i = jax.nn.sigmoid(g[1])
    log_a = -LRU_C * r * jax.nn.softplus(-lam.astype(jnp.float32))
    a = jnp.exp(log_a)
    b = jnp.sqrt(jnp.maximum(1.0 - jnp.exp(2.0 * log_a), 0.0)) * (i * uf)
    return a, b


def _combine(p, q):
    a1, b1 = p
    a2, b2 = q
    return a1 * a2, a2 * b1 + b2


def linear_scan(a, b, h0):
    a_cum, b_cum = lax.associative_scan(_combine, (a, b), axis=1)
    return a_cum * h0[:, None, :] + b_cum


def rglru_mixer(h_lat, h_ctx, w_in, conv_w, conv_b, gate_w, gate_b, lam, w_out, need_ctx):
    u_lat = h_lat @ w_in
    u_ctx = h_ctx @ w_in
    r_lat = centred_depthwise_conv(u_lat[..., LRU_WIDTH:], conv_w, conv_b)
    r_ctx = centred_depthwise_conv(u_ctx[..., LRU_WIDTH:], conv_w, conv_b)
    B = h_lat.shape[0]
    h0 = jnp.zeros((B, LRU_WIDTH), jnp.float32)
    y_lat = jnp.zeros(r_lat.shape, jnp.float32)
    y_ctx = jnp.zeros(r_ctx.shape, jnp.float32)
    for d in range(2):
        a_c, b_c = lru_coefficients(r_ctx, gate_w[d], gate_b[d], lam[d])
        a_l, b_l = lru_coefficients(r_lat, gate_w[d], gate_b[d], lam[d])
        if d == 1:
            a_c, b_c, a_l, b_l = [jnp.flip(t, axis=1) for t in (a_c, b_c, a_l, b_l)]
        s_c = linear_scan(a_c, b_c, h0)
        s_l = linear_scan(a_l, b_l, s_c[:, -1])
        if d == 1:
            s_c = jnp.flip(s_c, axis=1)
            s_l = jnp.flip(s_l, axis=1)
        y_lat = y_lat + s_l
        y_ctx = y_ctx + s_c
    out_lat = (y_lat.astype(h_lat.dtype) * jax.nn.gelu(u_lat[..., :LRU_WIDTH])) @ w_out
    out_ctx = None
    if need_ctx:
        out_ctx = (y_ctx.astype(h_ctx.dtype) * jax.nn.gelu(u_ctx[..., :LRU_WIDTH])) @ w_out
    return out_lat, out_ctx


def attend(q, k, v, mask, sink):
    s = jnp.einsum('bqhgd,bkhd->bhgqk', q, k).astype(jnp.float32)
    if mask is not None:
        s = jnp.where(mask, s, NEG_INF)
    sk = jnp.broadcast_to(sink.astype(jnp.float32)[None, :, :, None, None], s.shape[:-1] + (1,))
    p = jax.nn.softmax(jnp.concatenate([s, sk], axis=-1), axis=-1)[..., :-1]
    return jnp.einsum('bhgqk,bkhd->bqhgd', p.astype(v.dtype), v)


def attention_mixer(h_lat, h_ctx, w_qkv, sinks, w_o, row_pos, col_pos, need_ctx):
    B, S, _ = h_lat.shape
    C = h_ctx.shape[1]
    scale = HEAD_DIM ** -0.5
    sink_g = sinks.reshape(N_KV_HEADS, GROUP)
    qkv = h_lat @ w_qkv
    q_l = qkv[..., :Q_DIM].reshape(B, S, N_HEADS, HEAD_DIM)
    k_l = qkv[..., Q_DIM:Q_DIM + KV_DIM].reshape(B, S, N_KV_HEADS, HEAD_DIM)
    v_l = qkv[..., Q_DIM + KV_DIM:].reshape(B, S, N_KV_HEADS, HEAD_DIM)
    q_l = (rope_2d(q_l, row_pos, col_pos) * scale).reshape(B, S, N_KV_HEADS, GROUP, HEAD_DIM)
    k_l = rope_2d(k_l, row_pos, col_pos)
    kv_c = h_ctx @ w_qkv[:, Q_DIM:]
    k_c = kv_c[..., :KV_DIM].reshape(B, C, N_KV_HEADS, HEAD_DIM)
    v_c = kv_c[..., KV_DIM:].reshape(B, C, N_KV_HEADS, HEAD_DIM)

    nb = S // Q_BLOCK
    qb = jnp.moveaxis(q_l.reshape(B, nb, Q_BLOCK, N_KV_HEADS, GROUP, HEAD_DIM), 1, 0)

    def band(t):
        tp = jnp.pad(t, ((0, 0), (Q_BLOCK, Q_BLOCK), (0, 0), (0, 0)))
        tp = tp.reshape(B, nb + 2, Q_BLOCK, N_KV_HEADS, HEAD_DIM)
        tb = jnp.concatenate([tp[:, :-2], tp[:, 1:-1], tp[:, 2:]], axis=2)
        return jnp.moveaxis(tb, 1, 0)

    kb, vb = band(k_l), band(v_l)
    ctx_mask = jnp.ones((Q_BLOCK, C), dtype=bool)

    def block(args):
        q_j, k_j, v_j, j = args
        qpos = j * Q_BLOCK + jnp.arange(Q_BLOCK)
        kpos = (j - 1) * Q_BLOCK + jnp.arange(3 * Q_BLOCK)
        in_band = ((jnp.abs(qpos[:, None] - kpos[None, :]) <= WINDOW)
                   & (kpos >= 0)[None, :] & (kpos < S)[None, :])
        mask = jnp.concatenate([in_band, ctx_mask], axis=1)
        return attend(q_j, jnp.concatenate([k_j, k_c], axis=1),
                      jnp.concatenate([v_j, v_c], axis=1), mask, sink_g)

    o = lax.map(block, (qb, kb, vb, jnp.arange(nb)))
    out_lat = jnp.moveaxis(o, 0, 1).reshape(B, S, Q_DIM) @ w_o
    out_ctx = None
    if need_ctx:
        q_c = (h_ctx @ w_qkv[:, :Q_DIM]).reshape(B, C, N_KV_HEADS, GROUP, HEAD_DIM) * scale
        out_ctx = attend(q_c, k_c, v_c, None, sink_g).reshape(B, C, Q_DIM) @ w_o
    return out_lat, out_ctx


def moe_ffn(t, w_r, b_r, w_gu, b_gu, w_dn, b_dn):
    T, D = t.shape
    logits = (t @ w_r + b_r).astype(jnp.float32)
    top_v, top_e = lax.top_k(logits, TOP_K)
    gate = jax.nn.softmax(top_v, axis=-1)
    A = T * TOP_K
    flat_e = top_e.reshape(-1)
    flat_tok = jnp.arange(A) // TOP_K
    flat_g = gate.reshape(-1)
    order = jnp.argsort(flat_e)
    se, stok, sg = flat_e[order], flat_tok[order], flat_g[order]
    counts = jnp.bincount(flat_e, length=N_EXPERTS)
    padded = ((counts + EXPERT_BLOCK - 1) // EXPERT_BLOCK) * EXPERT_BLOCK
    start = jnp.cumsum(counts) - counts
    ends = jnp.cumsum(padded)
    pstart = ends - padded
    dest = pstart[se] + (jnp.arange(A) - start[se])
    P = (-(-A // EXPERT_BLOCK)) * EXPERT_BLOCK + N_EXPERTS * EXPERT_BLOCK
    n_blk = P // EXPERT_BLOCK
    row_tok = jnp.full((P,), T, dtype=jnp.int32).at[dest].set(stok.astype(jnp.int32))
    row_g = jnp.zeros((P,), jnp.float32).at[dest].set(sg)
    blk_e = jnp.minimum(jnp.searchsorted(ends, jnp.arange(n_blk) * EXPERT_BLOCK, side='right'),
                        N_EXPERTS - 1)
    t_pad = jnp.concatenate([t, jnp.zeros((1, D), t.dtype)], axis=0)
    xs = t_pad[row_tok].reshape(n_blk, EXPERT_BLOCK, D)

    def expert_block(args):
        xb, e = args
        h = xb @ w_gu[e] + b_gu[e]
        g = jnp.minimum(h[:, :D_EXPERT], SWIGLU_LIMIT)
        u = jnp.clip(h[:, D_EXPERT:], -SWIGLU_LIMIT, SWIGLU_LIMIT)
        y = (u + 1.0) * (g * jax.nn.sigmoid(SWIGLU_ALPHA * g))
        return y @ w_dn[e] + b_dn[e]

    out = lax.map(expert_block, (xs, blk_e)).reshape(P, D)
    out = out * row_g[:, None].astype(out.dtype)
    y = jnp.zeros((T + 1, D), out.dtype).at[row_tok].add(out)
    return y[:T]


def setup_inputs(seed: int = 0) -> dict:
    key = jax.random.key(seed)
    ks = jax.random.split(key, 24)
    D = D_MODEL
    f32 = jnp.float32

    def nrm(k, shape, s):
        return jax.random.normal(k, shape, f32) * s

    u = jax.random.uniform(ks[13], (N_LRU_LAYERS, 2, LRU_WIDTH), f32, 0.9, 0.999)
    a_base = u ** (1.0 / LRU_C)
    lru_lambda = jnp.log(a_base) - jnp.log1p(-a_base)
    return {
        'x': nrm(ks[0], (BATCH, SEQ, D), 1.0),
        'c': nrm(ks[1], (BATCH, D), 1.0),
        'ctx': nrm(ks[2], (BATCH, CTX_LEN, D), 1.0),
        'c_ctx': nrm(ks[3], (D,), 1.0),
        'ada_w': nrm(ks[4], (DEPTH, 2, D, 3 * D), 0.5 * D ** -0.5),
        'ada_b': nrm(ks[5], (DEPTH, 2, 3 * D), 0.02),
        'ln_g': 1.0 + nrm(ks[6], (DEPTH, 2, D), 0.02),
        'ln_b': nrm(ks[7], (DEPTH, 2, D), 0.02),
        'lru_w_in': nrm(ks[8], (N_LRU_LAYERS, D, 2 * LRU_WIDTH), D ** -0.5),
        'lru_conv_w': nrm(ks[9], (N_LRU_LAYERS, CONV_W, LRU_WIDTH), CONV_W ** -0.5),
        'lru_conv_b': nrm(ks[10], (N_LRU_LAYERS, LRU_WIDTH), 0.02),
        'lru_gate_w': nrm(ks[11], (N_LRU_LAYERS, 2, 2, LRU_BLOCKS, LRU_BLOCK_W, LRU_BLOCK_W), LRU_BLOCK_W ** -0.5),
        'lru_gate_b': nrm(ks[12], (N_LRU_LAYERS, 2, 2, LRU_WIDTH), 0.02),
        'lru_lambda': lru_lambda,
        'lru_w_out': nrm(ks[14], (N_LRU_LAYERS, LRU_WIDTH, D), LRU_WIDTH ** -0.5 * DN_BETA),
        'attn_w_qkv': nrm(ks[15], (N_ATTN_LAYERS, D, Q_DIM + 2 * KV_DIM), D ** -0.5),
        'attn_sinks': nrm(ks[16], (N_ATTN_LAYERS, N_HEADS), 0.5),
        'attn_w_o': nrm(ks[17], (N_ATTN_LAYERS, Q_DIM, D), Q_DIM ** -0.5 * DN_BETA),
        'router_w': nrm(ks[18], (DEPTH, D, N_EXPERTS), D ** -0.5),
        'router_b': nrm(ks[19], (DEPTH, N_EXPERTS), 0.01),
        'moe_w_gu': nrm(ks[20], (DEPTH, N_EXPERTS, D, 2 * D_EXPERT), D ** -0.5),
        'moe_b_gu': nrm(ks[21], (DEPTH, N_EXPERTS, 2 * D_EXPERT), 0.01),
        'moe_w_down': nrm(ks[22], (DEPTH, N_EXPERTS, D_EXPERT, D), D_EXPERT ** -0.5 * DN_BETA),
        'moe_b_down': nrm(ks[23], (DEPTH, N_EXPERTS, D), 0.01),
    }


def reference(x, c, ctx, c_ctx, ada_w, ada_b, ln_g, ln_b, lru_w_in, lru_conv_w, lru_conv_b,
              lru_gate_w, lru_gate_b, lru_lambda, lru_w_out, attn_w_qkv, attn_sinks, attn_w_o,
              router_w, router_b, moe_w_gu, moe_b_gu, moe_w_down, moe_b_down):
    B, S, D = x.shape
    C = ctx.shape[1]
    rows = S // GRID_W
    row_pos = jnp.repeat(jnp.arange(rows), GRID_W).astype(jnp.float32)
    col_pos = jnp.tile(jnp.arange(GRID_W), rows).astype(jnp.float32)
    c_act = jax.nn.silu(c)
    cc_act = jax.nn.silu(c_ctx)
    h_ctx = ctx
    for i in range(DEPTH):
        need_ctx = i < DEPTH - 1
        m_lat = (jnp.einsum('bd,sde->sbe', c_act, ada_w[i]) + ada_b[i][:, None, :])[:, :, None, :]
        m_ctx = (jnp.einsum('d,sde->se', cc_act, ada_w[i]) + ada_b[i])[:, None, None, :]

        sh_l, sc_l, g_l = jnp.split(m_lat[0], 3, axis=-1)
        sh_c, sc_c, g_c = jnp.split(m_ctx[0], 3, axis=-1)
        a_lat = x * (1.0 + sc_l) + sh_l
        a_ctx = h_ctx * (1.0 + sc_c) + sh_c
        j = i // N_MIXERS
        if i % N_MIXERS == 0:
            y_lat, y_ctx = rglru_mixer(a_lat, a_ctx, lru_w_in[j], lru_conv_w[j], lru_conv_b[j],
                                       lru_gate_w[j], lru_gate_b[j], lru_lambda[j], lru_w_out[j],
                                       need_ctx)
        else:
            y_lat, y_ctx = attention_mixer(a_lat, a_ctx, attn_w_qkv[j], attn_sinks[j], attn_w_o[j],
                                           row_pos, col_pos, need_ctx)
        x = layer_norm(DN_ALPHA * x + g_l * y_lat, ln_g[i, 0], ln_b[i, 0])
        if need_ctx:
            h_ctx = layer_norm(DN_ALPHA * h_ctx + g_c * y_ctx, ln_g[i, 0], ln_b[i, 0])

        sh_l, sc_l, g_l = jnp.split(m_lat[1], 3, axis=-1)
        sh_c, sc_c, g_c = jnp.split(m_ctx[1], 3, axis=-1)
        a_lat = (x * (1.0 + sc_l) + sh_l).reshape(B * S, D)
        if need_ctx:
            a_ctx = (h_ctx * (1.0 + sc_c) + sh_c).reshape(B * C, D)
            tokens = jnp.concatenate([a_lat, a_ctx], axis=0)
        else:
            tokens = a_lat
        y = moe_ffn(tokens, router_w[i], router_b[i], moe_w_gu[i], moe_b_gu[i],
                    moe_w_down[i], moe_b_down[i])
        x = layer_norm(DN_ALPHA * x + g_l * y[:B * S].reshape(B, S, D), ln_g[i, 1], ln_b[i, 1])
        if need_ctx:
            h_ctx = layer_norm(DN_ALPHA * h_ctx + g_c * y[B * S:].reshape(B, C, D),
                               ln_g[i, 1], ln_b[i, 1])
    return x
```

```python
import re
import numpy as np
from contextlib import ExitStack
import concourse.bass as bass
import concourse.mybir as mybir
from concourse.bass_utils import run_bass_kernel_spmd

F32 = mybir.dt.float32
BF16 = mybir.dt.bfloat16
U32 = mybir.dt.uint32
I32 = mybir.dt.int32
AF = mybir.ActivationFunctionType
ALU = mybir.AluOpType
AX = mybir.AxisListType

D = 2048
S = 2048
C = 256
T = S + C
NT = T // 128
DEPTH = 4
NE = 32
DE = 768
CAP = 384
DN_ALPHA = (2.0 * DEPTH) ** 0.25
LN_EPS = 1e-5
ENGS = ['pe', 'act', 'dve', 'pool', 'sp']


class Prog:
    def __init__(self, nc, n_dma_sems=40):
        self.nc = nc
        self.ops = {e: [] for e in ENGS}
        self.sem = {e: nc.alloc_semaphore(name=f"s_{e}") for e in ENGS}
        self.cnt = {e: 0 for e in ENGS}
        self.seen = {e: {} for e in ENGS}
        self.res = {}
        self.dsem = [nc.alloc_semaphore(name=f"s_dma{i}") for i in range(n_dma_sems + 28)]
        self.dval = [0] * (n_dma_sems + 28)
        self.drange = {'hw': (0, n_dma_sems), 'sw': (n_dma_sems, n_dma_sems + 28)}
        self.dnext = {'hw': 0, 'sw': n_dma_sems}
        self.nins = 0

    def _deps(self, r, w):
        waits = {}

        def add(ev):
            if ev is None:
                return
            s, v = ev
            k = id(s)
            if k not in waits or waits[k][1] < v:
                waits[k] = (s, v)
        for x in r:
            st = self.res.get(x)
            if st:
                add(st['w'])
        for x in w:
            st = self.res.get(x)
            if st:
                add(st['w'])
                for ev in st['r'].values():
                    add(ev)
        return waits

    def _record(self, ev, r, w):
        for x in w:
            self.res[x] = {'w': ev, 'r': {}}
        for x in r:
            st = self.res.setdefault(x, {'w': None, 'r': {}})
            k = id(ev[0])
            if k not in st['r'] or st['r'][k][1] < ev[1]:
                st['r'][k] = ev

    def op(self, eng, fn, r=(), w=(), dma=False):
        waits = self._deps(r, w)
        if dma:
            kind = 'sw' if eng == 'pool' else 'hw'
            lo, hi = self.drange[kind]
            i = self.dnext[kind]
            self.dnext[kind] = lo + (i + 1 - lo) % (hi - lo)
            s = self.dsem[i]
            if self.dval[i] > 0:
                waits[id(s)] = (s, self.dval[i])
            self.dval[i] += 16
            ev = (s, self.dval[i])
            incn = 16
        else:
            self.cnt[eng] += 1
            ev = (self.sem[eng], self.cnt[eng])
            incn = 1
        wl = []
        seen = self.seen[eng]
        for k, (s, v) in waits.items():
            if seen.get(k, 0) >= v:
                continue
            if eng == 'pe' and s is self.sem['pe']:
                continue
            seen[k] = v
            wl.append((s, v))
        self.ops[eng].append((wl, fn, ev[0], incn))
        self._record(ev, r, w)
        self.nins += 1
        return ev

    def raw(self, eng, fn):
        self.ops[eng].append(([], fn, None, 0))

    def barrier(self):
        evs = [(self.sem[e], self.cnt[e]) for e in ENGS if self.cnt[e] > 0]
        evs += [(s, v) for s, v in zip(self.dsem, self.dval) if v > 0]
        for e in ENGS:
            seen = self.seen[e]
            wl = []
            for (s, v) in evs:
                if seen.get(id(s), 0) >= v:
                    continue
                seen[id(s)] = v
                wl.append((s, v))
            if wl:
                self.ops[e].append((wl, None, None, 0))
        self.res = {}

    def finish(self):
        self.barrier()
        nc = self.nc
        with nc.Block() as block:
            def replay(e, lst):
                for (wl, fn, s, incn) in lst:
                    for (ws, wv) in wl:
                        e.wait_ge(ws, wv)
                    if fn is None:
                        continue
                    ins = fn(e)
                    if s is None:
                        continue
                    if isinstance(ins, (list, tuple)):
                        ins = ins[-1]
                    ins.then_inc(s, incn)

            @block.tensor
            def _(e):
                replay(e, self.ops['pe'])

            @block.scalar
            def _(e):
                replay(e, self.ops['act'])

            @block.vector
            def _(e):
                replay(e, self.ops['dve'])

            @block.gpsimd
            def _(e):
                replay(e, self.ops['pool'])

            @block.sync
            def _(e):
                replay(e, self.ops['sp'])


def sub_ap(ap, free):
    lst = [list(ap.ap[0])] + [[int(s), int(n)] for s, n in free]
    return bass.AP(ap.tensor, ap.offset, lst)


def rev_ap(ap2d):
    lst = [list(a) for a in ap2d.ap]
    st, n = lst[-1]
    lst[-1] = [-st, n]
    return bass.AP(ap2d.tensor, ap2d.offset + (n - 1) * st, lst)


class Builder:
    def __init__(self, plan, dbg=()):
        self.plan = plan
        self.nc = nc = bass.Bass("TRN2", target_bir_lowering=False)
        self.P = Prog(nc)
        self.dbg = set(dbg)
        self.din = {}
        self.uid = 0

    def inp(self, name, shape, dt=F32):
        h = self.nc.dram_tensor(name, list(shape), dt, kind="ExternalInput")
        self.din[name] = h
        return h.ap()

    def scratch(self, name, shape, dt=F32):
        kind = "ExternalOutput" if name in self.dbg else "Internal"
        return self.nc.dram_tensor(name, list(shape), dt, kind=kind).ap()

    def scratch_out(self, name, shape, dt=F32):
        return self.nc.dram_tensor(name, list(shape), dt, kind="ExternalOutput").ap()

    def free_dma_tmps(self, ins):
        g = self.nc.gpsimd
        RH = type(self.reg_off)
        for nm_ in set(re.findall(r"Pool_tmp_(\d+)", ins.concise())):
            n = int(nm_)
            g.free_register(RH(name=f"Pool_tmp_{n}", engine=self.reg_off.engine))
            for d_ in (1, 2, 3):
                try:
                    g.free_register(RH(name=f"Pool_Pool_moe_off_snap_{n - d_}", engine=self.reg_off.engine))
                    break
                except Exception:
                    pass

    def nm(self, p):
        self.uid += 1
        return f"{p}_{self.uid}"

    def dma(self, eng, out, in_, r, w):
        return self.P.op(eng, lambda e: e.dma_start(out=out, in_=in_), r=r, w=w, dma=True)

    SHAPES = {
        "x_in": [T, D], "cvec": [32, 128], "ident": [128, 128],
        "ada_w": [DEPTH, 2, D, 3 * D], "ada_b": [DEPTH, 2, 3 * D],
        "ln_g": [DEPTH, 2, D], "ln_b": [DEPTH, 2, D],
        "lru_w_in": [2, D, 2 * D], "lru_conv_w": [2, 4, D], "lru_conv_b": [2, D],
        "lru_gate_w": [2, 2, 2, 8, 256, 256], "lru_gate_b": [2, 2, 2, D], "lru_lambda": [2, 2, D],
        "lru_w_out": [2, D, D], "attn_w_qkv": [2, D, 2560], "attn_sinks": [2, 32], "attn_w_o": [2, D, D],
        "router_w": [DEPTH, D, NE], "router_b": [DEPTH, NE],
        "moe_w_gu": [DEPTH, NE, D, 2 * DE], "moe_b_gu": [DEPTH, NE, 2 * DE],
        "moe_w_down": [DEPTH, NE, DE, D], "moe_b_down": [DEPTH, NE, D],
        "rope_t": [S, 64], "tri": [3, 128, 128], "iota": [128, 512], "pcol": [128, 1],
    }

    def g(self, name):
        if name not in self.din:
            self.din[name] = self.nc.dram_tensor(name, list(self.SHAPES[name]), F32, kind="ExternalInput")
        return self.din[name].ap()

    def declare_io(self):
        nc = self.nc
        self.y_out = nc.dram_tensor("y_out", [S, D], F32, kind="ExternalOutput").ap()
        self.xs = self.scratch("xs", [T, D])
        self.modb = self.scratch("modb", [DEPTH, 2, 2, 128, 3 * D])
        self.uT = self.scratch("uT", [2 * D, T])
        self.mT = self.scratch("mT", [D, T], BF16)
        self.qTd = self.scratch("qTd", [NT, 128, 16, 128], BF16)
        self.xdisp = self.scratch("xdisp", [(T * 4 // 128 + NE) * 128, D], BF16)
        self.yslot = self.scratch("yslot", [(T * 4 // 128 + NE) * 128, D])

    def consts(self):
        nc, P = self.nc, self.P
        self.ident32 = nc.alloc_sbuf_tensor("ident32", [128, 128], F32)
        self.ident16 = nc.alloc_sbuf_tensor("ident16", [128, 128], BF16)
        self.ones32 = nc.alloc_sbuf_tensor("ones32", [128, 128], F32)
        self.ones16 = nc.alloc_sbuf_tensor("ones16", [128, 128], BF16)
        self.dma('sp', self.ident32[:], self.g('ident'), r=[], w=['ident32'])
        P.op('dve', lambda e: e.tensor_copy(out=self.ident16[:], in_=self.ident32[:]), r=['ident32'], w=['ident16'])
        P.op('pool', lambda e: e.memset(self.ones32[:], 1.0), w=['ones32'])
        P.op('pool', lambda e: e.memset(self.ones16[:], 1.0), w=['ones16'])
        self.reg_e = nc.gpsimd.alloc_register("moe_e")
        self.reg_off = nc.gpsimd.alloc_register("moe_off")
        self.eps_t = nc.alloc_sbuf_tensor("eps_t", [128, 1], F32)
        P.op('pool', lambda e: e.memset(self.eps_t[:], LN_EPS), w=['eps'])

    def bcast_rows(self, st, ps, rows, dst, tag):
        P = self.P
        N = dst.shape[-1]
        ps_ap, ps_res = ps
        for n0 in range(0, N, 512):
            n1 = min(N, n0 + 512)
            P.op('pe', lambda e, n0=n0, n1=n1: e.matmul(ps_ap[:, 0:n1 - n0], lhsT=self.ones32[0:1, :], rhs=rows[0:1, n0:n1], start=True, stop=True),
                 r=['ones32', tag + '_row'], w=[ps_res])
            P.op('act', lambda e, n0=n0, n1=n1: e.activation(out=dst[:, n0:n1], in_=ps_ap[:, 0:n1 - n0], func=AF.Copy),
                 r=[ps_res], w=[tag])

    def stage_ada(self, layers):
        nc, P = self.nc, self.P
        with ExitStack() as es:
            A = lambda *a: es.enter_context(nc.sbuf_tensor(self.nm(a[0]), *a[1:]))
            crow = A("ada_crow", [32, 128], F32)
            cT = A("ada_cT", [128, 32], F32)
            crep = A("ada_crep", [128, 2, 16, 128], BF16)
            wp = [A(f"ada_wp{i}", [128, 16, 512], BF16) for i in range(3)]
            brow = A("ada_brow", [1, 3 * D], BF16)
            stg = [A(f"ada_stg{i}", [128, 512], F32) for i in range(2)]
            ps = [es.enter_context(nc.psum_tensor(f"ada_ps{i}", [128, 512], F32)) for i in range(3)]
            self.dma('sp', crow[:], self.g('cvec'), r=[], w=['crow'])
            P.op('pe', lambda e: e.transpose(out=ps[2][:, 0:32], in_=crow[:], identity=self.ident32[0:32, 0:32]), r=['crow', 'ident32'], w=['aps2'])
            P.op('act', lambda e: e.activation(out=cT[:], in_=ps[2][:, 0:32], func=AF.Silu), r=['aps2'], w=['cT'])
            for v in range(2):
                src = sub_ap(cT[:, v * 16:(v + 1) * 16], [(1, 16), (0, 128)])
                P.op('dve', lambda e, v=v, src=src: e.tensor_copy(out=crep[:, v, :, :], in_=src), r=['cT'], w=[f'crep{v}'])
            k = 0
            kw = 0
            for i in layers:
                for s in range(2):
                    for h3 in range(3):
                        P.op('pool', lambda e, i=i, s=s, h3=h3: e.dma_start(out=brow[0:1, h3 * D:(h3 + 1) * D], in_=self.g('ada_b')[i, s:s + 1, h3 * D:(h3 + 1) * D]), r=[], w=[f'brow{h3}'], dma=True)
                    wsrc = self.g('ada_w')[i, s].rearrange("(c p) n -> p c n", p=128)
                    for pn in range(12):
                        wb = wp[kw % 3]
                        wr = f'wp{kw % 3}'
                        kw += 1
                        for h in range(4):
                            P.op('pool', lambda e, wb=wb, h=h, pn=pn, wsrc=wsrc: e.dma_start(out=wb[:, 4 * h:4 * h + 4, :], in_=wsrc[:, 4 * h:4 * h + 4, pn * 512:(pn + 1) * 512]), r=[], w=[wr + f'_{h}'], dma=True)
                        for v in range(2):
                            pt = ps[k % 2]
                            pr = f'aps{k % 2}'
                            for c in range(16):
                                P.op('pe', lambda e, pt=pt, wb=wb, v=v, c=c: e.matmul(pt[:], lhsT=crep[:, v, c, :], rhs=wb[:, c, :], start=(c == 0), stop=False),
                                     r=[f'crep{v}', wr + f'_{c // 4}'], w=[pr])
                            P.op('pe', lambda e, pt=pt, pn=pn: e.matmul(pt[:], lhsT=self.ones16[0:1, :], rhs=brow[0:1, pn * 512:(pn + 1) * 512], start=False, stop=True),
                                 r=['ones16'] + [f'brow{h3}' for h3 in range(3)], w=[pr])
                            sg = stg[k % 2]
                            sr = f'astg{k % 2}'
                            bias = 1.0 if 4 <= pn < 8 else 0.0
                            P.op('act', lambda e, sg=sg, pt=pt, bias=bias: e.activation(out=sg[:], in_=pt[:], func=AF.Identity, bias=bias, scale=1.0), r=[pr], w=[sr])
                            self.dma('sp', self.modb[i, s, v, :, pn * 512:(pn + 1) * 512], sg[:], r=[sr], w=[f'modb{i}{s}{v}'])
                            k += 1
            P.barrier()


    def ln_alloc(self, es, pfx):
        nc = self.nc
        A = lambda *a: es.enter_context(nc.sbuf_tensor(self.nm(a[0]), *a[1:]))
        L = {}
        L['lng'] = A(pfx + "_lng", [128, D], F32)
        L['lnb'] = A(pfx + "_lnb", [128, D], F32)
        L['row'] = A(pfx + "_row", [1, D], F32)
        L['st'] = [A(pfx + f"_st{i}", [128, 4, 6], F32) for i in range(2)]
        L['mv'] = [A(pfx + f"_mv{i}", [128, 8], F32) for i in range(2)]
        L['xn'] = [A(pfx + f"_xn{i}", [128, D], F32) for i in range(2)]
        L['k'] = 0
        return L

    def ln_load(self, L, i, s, ps):
        for nm, src in (('lng', self.g('ln_g')), ('lnb', self.g('ln_b'))):
            self.dma('sp', L['row'][:], src[i, s:s + 1, :], r=[], w=['ln_row'])
            self.bcast_rows(None, ps, L['row'], L[nm], 'ln')

    def ln_tile(self, L, pre, pre_res, dst, dst_res):
        P = self.P
        k = L['k'] % 2
        L['k'] += 1
        st, mv, xn = L['st'][k], L['mv'][k], L['xn'][k]
        sr, mr, xr = f'ln_st{k}', f'ln_mv{k}', f'ln_xn{k}'
        P.op('dve', lambda e: [e.bn_stats(out=st[:, j, :], in_=pre[:, j * 512:(j + 1) * 512]) for j in range(4)], r=[pre_res], w=[sr])
        P.op('dve', lambda e: e.bn_aggr(out=mv[:, 0:2], in_=st[:].rearrange("p a b -> p (a b)")), r=[sr], w=[mr])
        P.op('act', lambda e: e.activation(out=mv[:, 2:3], in_=mv[:, 1:2], func=AF.Sqrt, bias=self.eps_t[:, 0:1], scale=1.0), r=[mr, 'eps'], w=[mr + 'a'])
        P.op('dve', lambda e: e.reciprocal(out=mv[:, 3:4], in_=mv[:, 2:3]), r=[mr + 'a'], w=[mr + 'b'])
        P.op('dve', lambda e: e.tensor_scalar(out=mv[:, 4:5], in0=mv[:, 0:1], scalar1=mv[:, 3:4], scalar2=-1.0, op0=ALU.mult, op1=ALU.mult), r=[mr, mr + 'b'], w=[mr + 'c'])
        P.op('act', lambda e: e.activation(out=xn[:], in_=pre[:], func=AF.Identity, bias=mv[:, 4:5], scale=mv[:, 3:4]), r=[pre_res, mr + 'b', mr + 'c'], w=[xr])
        P.op('dve', lambda e: e.tensor_tensor(out=xn[:], in0=xn[:], in1=L['lng'][:], op=ALU.mult), r=['ln'], w=[xr])
        P.op('dve', lambda e: e.tensor_tensor(out=xn[:], in0=xn[:], in1=L['lnb'][:], op=ALU.add), r=['ln'], w=[xr])
        self.dma('sp', dst, xn[:], r=[xr], w=[dst_res])

    def stage_moe(self, i, srcname, dstname, need_ctx):
        nc, P = self.nc, self.P
        src = self.g('x_in') if srcname == 'x_in' else self.xs
        nt = NT if need_ctx else S // 128
        NBLK = (nt * 128 * 4) // 128 + NE
        BIG = float(DEPTH * NE)
        wgu_t = self.g('moe_w_gu').tensor
        wdn_t = self.g('moe_w_down').tensor
        bgu_t = self.g('moe_b_gu').tensor
        with ExitStack() as es0:
            A0 = lambda *a: es0.enter_context(nc.sbuf_tensor(self.nm(a[0]), *a[1:]))
            Sk = A0("moe_Sk", [128, NT * 4], I32)
            Gk = A0("moe_Gk", [128, NT * 4], F32)
            GD = A0("moe_GD", [128, NT, NE], F32)
            EIDX = A0("moe_eidx", [128, NBLK], I32)
            with ExitStack() as es:
                A = lambda *a: es.enter_context(nc.sbuf_tensor(self.nm(a[0]), *a[1:]))
                PS = lambda n, sh, dt: es.enter_context(nc.psum_tensor(self.nm(n), sh, dt))
                sc1 = [A(f"m1_sc{v}", [128, D], F32) for v in range(2)]
                shb = [A(f"m1_sh{v}", [128, D], F32) for v in range(2)]
                wr = A("m1_wr", [128, 16, NE], F32)
                brow = A("m1_brow", [1, NE], F32)
                brb = A("m1_brb", [128, NE], F32)
                iob = A("m1_iob", [128, NBLK], F32)
                pcol = A("m1_pcol", [128, 1], F32)
                tri32 = A("m1_tri32", [128, 128], F32)
                U16 = A("m1_U16", [128, 128], BF16)
                cm = A("m1_cm", [128, NE], F32)
                cm16 = A("m1_cm16", [128, NE], BF16)
                zer = A("m1_zer", [128, NE], F32)
                xt = [A(f"m1_xt{k}", [128, D], F32) for k in range(2)]
                a32 = [A(f"m1_a32{k}", [128, D], F32) for k in range(2)]
                a16 = A("m1_a16", [128, NT, D], BF16)
                aT = [A(f"m1_aT{k}", [128, 16, 128], F32) for k in range(2)]
                MK = A("m1_MK", [128, NT, NE], F32)
                PR = A("m1_PR", [128, NT, NE], F32)
                sm = [A(f"m1_sm{k}", [128, 8, NE], F32) for k in range(2)]
                m16 = [A(f"m1_m16{k}", [128, NE], BF16) for k in range(2)]
                sk = [A(f"m1_sk{k}", [128, 16], F32) for k in range(2)]
                fidx = A("m1_fidx", [128, NBLK, 16], F32)
                coff = A("m1_coff", [128, 16], F32)
                cw = A("m1_cw", [128, 6, NE], F32)
                cwi = A("m1_cwi", [128, NE], I32)
                eb = A("m1_eb", [128, 6, NBLK], F32)
                pT = [PS(f"m1_pT{k}", [128, 512], F32) for k in range(2)]
                pr = PS("m1_pr", [128, 512], F32)
                pp = PS("m1_pp", [128, 512], F32)
                for v in range(2):
                    self.dma('sp', sc1[v][:], self.modb[i, 1, v, :, D:2 * D], r=[], w=[f'sc1{v}'])
                    self.dma('act', shb[v][:], self.modb[i, 1, v, :, 0:D], r=[], w=[f'shb{v}'])
                self.dma('sp', wr[:], self.g('router_w')[i].rearrange("(c p) e -> p c e", p=128), r=[], w=['wr'])
                self.dma('sp', brow[:], self.g('router_b')[i:i + 1, :], r=[], w=['m1_row'])
                self.bcast_rows(None, (pr, 'pr'), brow, brb, 'm1')
                self.dma('sp', iob[:], self.g('iota')[:, 0:NBLK], r=[], w=['iob'])
                P.op('dve', lambda e: e.tensor_scalar(out=iob[:], in0=iob[:], scalar1=128.0, scalar2=None, op0=ALU.mult), r=[], w=['iob'])
                self.dma('sp', coff[:], self.g('iota')[:, 0:16], r=[], w=['coff'])
                P.op('dve', lambda e: e.tensor_scalar(out=coff[:], in0=coff[:], scalar1=128.0, scalar2=None, op0=ALU.mult), r=[], w=['coff'])
                self.dma('sp', pcol[:], self.g('pcol'), r=[], w=['pcol'])
                self.dma('sp', tri32[:], self.g('tri')[0], r=[], w=['tri32'])
                P.op('dve', lambda e: e.tensor_copy(out=U16[:], in_=tri32[:]), r=['tri32'], w=['U16'])
                P.op('pool', lambda e: e.memset(cm[:], 0.0), w=['cm'])
                P.op('pool', lambda e: e.memset(cm16[:], 0.0), w=['cm16'])
                P.op('pool', lambda e: e.memset(zer[:], 0.0), w=['zer'])
                for tt in range(nt):
                    v = 0 if tt < 16 else 1
                    k = tt % 2
                    X, A32, AT, SM, M16 = xt[k], a32[k], aT[k], sm[k], m16[k]
                    rx, ra32, raT, rsm = f'xt{k}', f'a32{k}', f'aT{k}', f'sm{k}'
                    self.dma('sp', X[:], src[tt * 128:(tt + 1) * 128, :], r=[f'xs{tt}'], w=[rx])
                    P.op('dve', lambda e, X=X, A32=A32, v=v: e.tensor_tensor(out=A32[:], in0=X[:], in1=sc1[v][:], op=ALU.mult), r=[rx, f'sc1{v}'], w=[ra32])
                    P.op('dve', lambda e, A32=A32, v=v: e.tensor_tensor(out=A32[:], in0=A32[:], in1=shb[v][:], op=ALU.add), r=[f'shb{v}'], w=[ra32])
                    P.op('act', lambda e, A32=A32, tt=tt: e.activation(out=a16[:, tt, :], in_=A32[:], func=AF.Copy), r=[ra32], w=[f'a16_{tt}'])
                    for q in range(4):
                        pt = pT[q % 2]
                        ptr = f'pT{q % 2}'
                        P.op('pe', lambda e, pt=pt, A32=A32, q=q: [e.transpose(out=pt[:, j * 128:(j + 1) * 128], in_=A32[:, (4 * q + j) * 128:(4 * q + j + 1) * 128], identity=self.ident32[:]) for j in range(4)],
                             r=[ra32, 'ident32'], w=[ptr])
                        dst = AT[:, 4 * q:4 * q + 4, :].rearrange("p a b -> p (a b)")
                        if q % 2 == 0:
                            P.op('act', lambda e, pt=pt, dst=dst: e.activation(out=dst, in_=pt[:], func=AF.Copy), r=[ptr], w=[raT + f'_{q}'])
                        else:
                            P.op('dve', lambda e, pt=pt, dst=dst: e.tensor_copy(out=dst, in_=pt[:]), r=[ptr], w=[raT + f'_{q}'])
                    P.op('pe', lambda e, AT=AT: [e.matmul(pr[:, 0:NE], lhsT=AT[:, c, :], rhs=wr[:, c, :], start=(c == 0), stop=(c == 15)) for c in range(16)],
                         r=[raT + f'_{q}' for q in range(4)] + ['wr'], w=['pr'])
                    lg, ex, exm = [SM[:, j, :] for j in range(3)]
                    mask = MK[:, tt, :]
                    gate = GD[:, tt, :]
                    mx8 = SM[:, 3, 0:8]
                    negm = SM[:, 3, 8:9]
                    ssum = SM[:, 3, 9:10]
                    rsum = SM[:, 3, 10:11]
                    rt = f'tok{tt}'
                    P.op('dve', lambda e, lg=lg: e.tensor_tensor(out=lg, in0=pr[:, 0:NE], in1=brb[:], op=ALU.add), r=['pr', 'm1'], w=[rsm])
                    P.op('dve', lambda e, lg=lg, mx8=mx8: e.max(out=mx8, in_=lg), r=[rsm], w=[rsm])
                    P.op('dve', lambda e, lg=lg, mx8=mx8, mask=mask: e.tensor_scalar(out=mask, in0=lg, scalar1=mx8[:, 3:4], scalar2=None, op0=ALU.is_ge), r=[rsm], w=[rt])
                    P.op('dve', lambda e, mx8=mx8, negm=negm: e.tensor_scalar(out=negm, in0=mx8[:, 0:1], scalar1=-1.0, scalar2=None, op0=ALU.mult), r=[rsm], w=[rsm])
                    P.op('act', lambda e, lg=lg, ex=ex, negm=negm: e.activation(out=ex, in_=lg, func=AF.Exp, bias=negm, scale=1.0), r=[rsm], w=[rsm])
                    P.op('dve', lambda e, ex=ex, mask=mask, exm=exm, ssum=ssum: e.scalar_tensor_tensor(out=exm, in0=ex, scalar=1.0, in1=mask, op0=ALU.mult, op1=ALU.mult, accum_out=ssum), r=[rsm, rt], w=[rsm])
                    P.op('dve', lambda e, ssum=ssum, rsum=rsum: e.reciprocal(out=rsum, in_=ssum), r=[rsm], w=[rsm])
                    P.op('dve', lambda e, exm=exm, gate=gate, rsum=rsum: e.tensor_scalar(out=gate, in0=exm, scalar1=rsum, scalar2=None, op0=ALU.mult), r=[rsm], w=[rt])
                    P.op('dve', lambda e, mask=mask, M16=M16: e.tensor_copy(out=M16[:], in_=mask), r=[rt], w=[f'm16{k}'])
                    P.op('pe', lambda e, M16=M16: [e.matmul(pp[:, 0:NE], lhsT=U16[:], rhs=M16[:], start=True, stop=False),
                                                   e.matmul(pp[:, 0:NE], lhsT=self.ones16[:], rhs=cm16[:], start=False, stop=True)],
                         r=['U16', f'm16{k}', 'ones16', 'cm16'], w=['pp'])
                    P.op('dve', lambda e, tt=tt: e.tensor_copy(out=PR[:, tt, :], in_=pp[:, 0:NE]), r=['pp'], w=[rt])
                    P.op('dve', lambda e, mask=mask: e.tensor_tensor(out=cm[:], in0=cm[:], in1=mask, op=ALU.add), r=[rt], w=['cm'])
                    P.op('dve', lambda e: e.tensor_copy(out=cm16[:], in_=cm[:]), r=['cm'], w=['cm16'])
                cnt, pad, ends, pst = [cw[:, j, :] for j in range(4)]
                P.op('pe', lambda e: e.matmul(pp[:, 0:NE], lhsT=self.ones16[:], rhs=cm16[:], start=True, stop=True), r=['ones16', 'cm16'], w=['pp'])
                P.op('dve', lambda e: e.tensor_scalar(out=cnt, in0=pp[:, 0:NE], scalar1=127.0, scalar2=None, op0=ALU.add), r=['pp'], w=['cw'])
                P.op('dve', lambda e: e.tensor_copy(out=cwi[:], in_=cnt), r=['cw'], w=['cwi'])
                P.op('dve', lambda e: e.tensor_single_scalar(out=cwi[:], in_=cwi[:], scalar=7, op=ALU.arith_shift_right), r=[], w=['cwi'])
                P.op('dve', lambda e: e.tensor_single_scalar(out=cwi[:], in_=cwi[:], scalar=7, op=ALU.logical_shift_left), r=[], w=['cwi'])
                P.op('dve', lambda e: e.tensor_copy(out=pad, in_=cwi[:]), r=['cwi'], w=['cw'])
                P.op('dve', lambda e: e.tensor_tensor_scan(out=ends, data0=pad, data1=zer[:], initial=0.0, op0=ALU.add, op1=ALU.add), r=['zer'], w=['cw'])
                P.op('dve', lambda e: e.tensor_tensor(out=pst, in0=ends, in1=pad, op=ALU.subtract), r=[], w=['cw'])
                Eb, Em2, need, bas, tmp = [eb[:, j, :] for j in range(5)]
                P.op('pool', lambda e: e.memset(eb[:], 0.0), w=['eb'])
                for ee in range(NE):
                    P.op('dve', lambda e, ee=ee: e.scalar_tensor_tensor(out=Eb, in0=iob[:], scalar=cw[:, 2, ee:ee + 1], in1=Eb, op0=ALU.is_ge, op1=ALU.add), r=['iob', 'cw'], w=['eb'])
                P.op('dve', lambda e: e.tensor_scalar(out=Eb, in0=Eb, scalar1=float(NE - 1), scalar2=None, op0=ALU.min), r=[], w=['eb'])
                P.op('dve', lambda e: e.memset(need, 1.0), w=['eb'])
                P.op('dve', lambda e: e.tensor_tensor(out=eb[:, 2, 1:NBLK], in0=eb[:, 0, 1:NBLK], in1=eb[:, 0, 0:NBLK - 1], op=ALU.not_equal), r=[], w=['eb'])
                P.op('dve', lambda e: e.memset(eb[:, 2, NBLK // 2:NBLK // 2 + 1], 1.0), w=['eb'])

                def mk_index(dst_i32, nchunk, rows_per_e, with_p):
                    P.op('dve', lambda e: e.tensor_scalar(out=bas, in0=Eb, scalar1=float(rows_per_e), scalar2=float(i * NE * rows_per_e) - BIG, op0=ALU.mult, op1=ALU.add), r=[], w=['eb'])
                    if with_p:
                        P.op('dve', lambda e: e.tensor_scalar(out=bas, in0=bas, scalar1=pcol[:, 0:1], scalar2=None, op0=ALU.add), r=['pcol'], w=['eb'])
                    P.op('dve', lambda e: e.tensor_tensor(out=bas, in0=bas, in1=need, op=ALU.mult), r=[], w=['eb'])
                    P.op('dve', lambda e: e.tensor_scalar(out=bas, in0=bas, scalar1=BIG, scalar2=None, op0=ALU.add), r=[], w=['eb'])
                    if nchunk == 1:
                        P.op('dve', lambda e: e.tensor_copy(out=dst_i32[:], in_=bas), r=['eb'], w=['idx'])
                    else:
                        fv = fidx[:, :, 0:nchunk]
                        P.op('dve', lambda e: e.tensor_tensor(out=fv, in0=sub_ap(bas, [(1, NBLK), (0, nchunk)]), in1=sub_ap(coff[:, 0:nchunk], [(0, NBLK), (1, nchunk)]), op=ALU.add), r=['coff'], w=['fidx'])
                        P.op('dve', lambda e: e.tensor_copy(out=dst_i32[:], in_=fv), r=['fidx'], w=['idx'])
                mk_index(EIDX, 1, 1, False)
                for tt in range(nt):
                    k = tt % 2
                    SM, SKF = sm[k], sk[k]
                    rsm = f'smb{k}'
                    rt = f'tok{tt}'
                    mask = MK[:, tt, :]
                    gate = GD[:, tt, :]
                    sful, cs, oh, junk = [SM[:, j, :] for j in range(4, 8)]
                    P.op('dve', lambda e, sful=sful, tt=tt: e.tensor_tensor(out=sful, in0=PR[:, tt, :], in1=pst, op=ALU.add), r=[rt, 'cw'], w=[rsm])
                    P.op('dve', lambda e, cs=cs, mask=mask: e.tensor_tensor_scan(out=cs, data0=mask, data1=zer[:], initial=0.0, op0=ALU.add, op1=ALU.add), r=[rt, 'zer'], w=[rsm])
                    for kk in range(4):
                        P.op('dve', lambda e, oh=oh, cs=cs, mask=mask, kk=kk: e.scalar_tensor_tensor(out=oh, in0=cs, scalar=float(kk + 1), in1=mask, op0=ALU.is_equal, op1=ALU.mult), r=[rsm], w=[rsm])
                        P.op('dve', lambda e, oh=oh, sful=sful, junk=junk, SKF=SKF, kk=kk: e.scalar_tensor_tensor(out=junk, in0=oh, scalar=1.0, in1=sful, op0=ALU.mult, op1=ALU.mult, accum_out=SKF[:, kk:kk + 1]), r=[rsm], w=[rsm, f'skf{k}'])
                        P.op('dve', lambda e, oh=oh, gate=gate, junk=junk, kk=kk, tt=tt: e.scalar_tensor_tensor(out=junk, in0=oh, scalar=1.0, in1=gate, op0=ALU.mult, op1=ALU.mult, accum_out=Gk[:, tt * 4 + kk:tt * 4 + kk + 1]), r=[rsm], w=[rsm, 'Gk'])
                    P.op('dve', lambda e, SKF=SKF, tt=tt: e.tensor_copy(out=Sk[:, tt * 4:tt * 4 + 4], in_=SKF[:, 0:4]), r=[f'skf{k}'], w=[f'Sk{tt}'])
                    for kk in range(4):
                        P.op('pool', lambda e, tt=tt, kk=kk: e.indirect_dma_start(out=self.xdisp, out_offset=bass.IndirectOffsetOnAxis(ap=Sk[:, tt * 4 + kk:tt * 4 + kk + 1], axis=0), in_=a16[:, tt, :], in_offset=None),
                             r=[f'a16_{tt}', f'Sk{tt}'], w=[], dma=True)
                if 'dbg_moe' in self.dbg:
                    self.dbg_sk = self.scratch_out("dbg_sk", [128, NT * 4], I32)
                    self.dbg_eb = self.scratch_out("dbg_eb", [128, 6, NBLK], F32)
                    self.dbg_cw = self.scratch_out("dbg_cw", [128, 6, NE], F32)
                    self.dbg_ig = self.scratch_out("dbg_ig", [128, NBLK], I32)
                    self.dma('sp', self.dbg_sk, Sk[:], r=[f'Sk{t_}' for t_ in range(nt)], w=[])
                    self.dma('sp', self.dbg_eb, eb[:], r=['eb'], w=[])
                    self.dma('sp', self.dbg_cw, cw[:], r=['cw'], w=[])
                    self.dma('sp', self.dbg_ig, EIDX[:], r=['idx'], w=[])
                P.barrier()
            with ExitStack() as es:
                A = lambda *a: es.enter_context(nc.sbuf_tensor(self.nm(a[0]), *a[1:]))
                PS = lambda n, sh, dt: es.enter_context(nc.psum_tensor(self.nm(n), sh, dt))
                WGU = [A(f"m2_wgu{k}", [128, 16, 2 * DE], BF16) for k in range(2)]
                WDN = [A(f"m2_wdn{k}", [128, 6, D], BF16) for k in range(2)]
                BG = [A(f"m2_bg{k}", [128, 2 * DE], BF16) for k in range(2)]
                xin = [A(f"m2_xin{k}", [128, D], BF16) for k in range(2)]
                xT = [A(f"m2_xT{k}", [128, 16, 128], BF16) for k in range(2)]
                g32 = [A(f"m2_g32{k}", [128, DE], F32) for k in range(2)]
                sg = [A(f"m2_sg{k}", [128, DE], F32) for k in range(2)]
                u1 = [A(f"m2_u1{k}", [128, DE], F32) for k in range(2)]
                y16 = [A(f"m2_y16{k}", [128, DE], BF16) for k in range(2)]
                yT = [A(f"m2_yT{k}", [128, 6, 128], BF16) for k in range(2)]
                ostg = [A(f"m2_ostg{k}", [128, 1024], F32) for k in range(2)]
                pTb = [PS(f"m2_pTb{k}", [128, 1024], BF16) for k in range(2)]
                ph = [PS(f"m2_ph{k}", [128, 512], F32) for k in range(4)]
                pd = [PS(f"m2_pd{k}", [128, 512], F32) for k in range(2)]
                HALF = NBLK // 2

                def front(b):
                    par = b % 2
                    k = b % 2
                    sb = (b % 2) * HALF + b // 2
                    Wg, Bg, XIN, XT = WGU[par], BG[par], xin[k], xT[k]
                    Wd = WDN[par]
                    G32, SG, U1, Y16 = g32[k], sg[k], u1[k], y16[k]
                    rwg, rwd, rbg = f'wgu{par}', f'wdn{par}', f'bg{par}'
                    self.dma('sp', XIN[:], self.xdisp[sb * 128:(sb + 1) * 128, :], r=[], w=[f'xin{k}'])

                    def ld(e, b=b, out=None, tensor=None, per_e=0, pat=None):
                        e.reg_load(self.reg_e, EIDX[0:1, sb:sb + 1])
                        e.reg_mul(self.reg_off, self.reg_e, per_e)
                        ins = e.dma_start(out=out, in_=bass.AP(tensor, self.reg_off, pat), bounds_check="skip_entire_dma")
                        self.free_dma_tmps(ins)
                        return ins
                    for qq in range(4):
                        def ldq(e, qq=qq):
                            e.reg_load(self.reg_e, EIDX[0:1, sb:sb + 1])
                            e.reg_mul(self.reg_off, self.reg_e, D * 2 * DE)
                            e.reg_add(self.reg_off, self.reg_off, qq * 4 * 128 * 2 * DE)
                            ins = e.dma_start(out=Wg[:, 4 * qq:4 * qq + 4, :], in_=bass.AP(wgu_t, self.reg_off, [[2 * DE, 128], [128 * 2 * DE, 4], [1, 2 * DE]]), bounds_check="skip_entire_dma")
                            self.free_dma_tmps(ins)
                            return ins
                        P.op('pool', ldq, r=[], w=[rwg + f'_{qq}'], dma=True)
                    P.op('pool', lambda e: ld(e, b, Bg[0:1, :], bgu_t, 2 * DE, [[2 * DE, 1], [1, 2 * DE]]), r=[], w=[rbg], dma=True)
                    P.op('pool', lambda e: ld(e, b, Wd[:, :, :], wdn_t, DE * D, [[D, 128], [128 * D, 6], [1, D]]), r=[], w=[rwd], dma=True)
                    for hh in range(2):
                        pt = pTb[hh]
                        ptr = f'pTb{hh}'
                        P.op('pe', lambda e, pt=pt, hh=hh: [e.transpose(out=pt[:, j * 128:(j + 1) * 128], in_=XIN[:, (8 * hh + j) * 128:(8 * hh + j + 1) * 128], identity=self.ident16[:]) for j in range(8)],
                             r=[f'xin{k}', 'ident16'], w=[ptr])
                        dst = XT[:, 8 * hh:8 * hh + 8, :].rearrange("p a b -> p (a b)")
                        if hh == 0:
                            P.op('act', lambda e, dst=dst, pt=pt: e.activation(out=dst, in_=pt[:], func=AF.Copy), r=[ptr], w=[f'xT{k}_{hh}'])
                        else:
                            P.op('dve', lambda e, dst=dst, pt=pt: e.tensor_copy(out=dst, in_=pt[:]), r=[ptr], w=[f'xT{k}_{hh}'])
                    segs = [(ph[0], 0, 512, 0, 'ph0'), (ph[1], 0, 512, DE, 'ph1'), (ph[2], 0, 256, 512, 'ph2'), (ph[3], 0, 256, DE + 512, 'ph3')]

                    def gu_group(e, qq):
                        out = []
                        for c in range(4 * qq, 4 * qq + 4):
                            for (pt_, o0, n_, w0, _) in segs:
                                out.append(e.matmul(pt_[:, o0:o0 + n_], lhsT=XT[:, c, :], rhs=Wg[:, c, w0:w0 + n_], start=(c == 0), stop=False))
                        if qq == 3:
                            for (pt_, o0, n_, w0, _) in segs:
                                out.append(e.matmul(pt_[:, o0:o0 + n_], lhsT=self.ones16[0:1, :], rhs=Bg[0:1, w0:w0 + n_], start=False, stop=True))
                        return out
                    for qq in range(4):
                        P.op('pe', lambda e, qq=qq: gu_group(e, qq), r=[f'xT{k}_0', f'xT{k}_1', 'ones16', rwg + f'_{qq}'] + ([rbg] if qq == 3 else []), w=['ph0', 'ph1', 'ph2', 'ph3'])
                    P.op('dve', lambda e: e.tensor_scalar(out=G32[:, 0:512], in0=ph[0][:], scalar1=7.0, scalar2=None, op0=ALU.min), r=['ph0'], w=[f'g32a{k}'])
                    P.op('dve', lambda e: e.tensor_scalar(out=G32[:, 512:DE], in0=ph[2][:, 0:256], scalar1=7.0, scalar2=None, op0=ALU.min), r=['ph2'], w=[f'g32b{k}'])
                    P.op('dve', lambda e: e.tensor_scalar(out=U1[:, 0:512], in0=ph[1][:], scalar1=7.0, scalar2=-7.0, op0=ALU.min, op1=ALU.max), r=['ph1'], w=[f'u1a{k}'])
                    P.op('dve', lambda e: e.tensor_scalar(out=U1[:, 512:DE], in0=ph[3][:, 0:256], scalar1=7.0, scalar2=-7.0, op0=ALU.min, op1=ALU.max), r=['ph3'], w=[f'u1b{k}'])
                    P.op('act', lambda e: e.activation(out=SG[:], in_=G32[:], func=AF.Sigmoid, scale=1.702), r=[f'g32a{k}', f'g32b{k}'], w=[f'sg{k}'])
                    P.op('dve', lambda e: e.tensor_tensor(out=SG[:], in0=SG[:], in1=G32[:], op=ALU.mult), r=[f'g32a{k}', f'g32b{k}'], w=[f'sg{k}'])
                    P.op('dve', lambda e: e.scalar_tensor_tensor(out=Y16[:], in0=U1[:], scalar=1.0, in1=SG[:], op0=ALU.add, op1=ALU.mult), r=[f'u1a{k}', f'u1b{k}', f'sg{k}'], w=[f'y16{k}'])

                def back(b):
                    par = b % 2
                    k = b % 2
                    sb = (b % 2) * HALF + b // 2
                    Wd, YT, Y16 = WDN[par], yT[k], y16[k]
                    rwd = f'wdn{par}'
                    P.op('pe', lambda e: [e.transpose(out=pTb[0][:, j * 128:(j + 1) * 128], in_=Y16[:, j * 128:(j + 1) * 128], identity=self.ident16[:]) for j in range(6)],
                         r=[f'y16{k}', 'ident16'], w=['pTb0'])
                    P.op('act', lambda e: e.activation(out=YT[:, :, :].rearrange("p a b -> p (a b)"), in_=pTb[0][:, 0:DE], func=AF.Copy), r=['pTb0'], w=[f'yT{k}'])
                    for hf in range(2):
                        OS = ostg[hf]
                        for q in range(2):
                            np_ = hf * 2 + q
                            pdt = pd[q]
                            pdr = f'pd{q}'
                            P.op('pe', lambda e, pdt=pdt, np_=np_: [e.matmul(pdt[:], lhsT=YT[:, j, :], rhs=Wd[:, j, np_ * 512:(np_ + 1) * 512], start=(j == 0), stop=(j == 5)) for j in range(6)],
                                 r=[f'yT{k}', rwd], w=[pdr])
                            P.op('act', lambda e, OS=OS, pdt=pdt, q=q: e.activation(out=OS[:, q * 512:(q + 1) * 512], in_=pdt[:], func=AF.Copy), r=[pdr], w=[f'ostg{hf}'])
                        self.dma('sp', self.yslot[sb * 128:(sb + 1) * 128, hf * 1024:(hf + 1) * 1024], OS[:], r=[f'ostg{hf}'], w=[])

                for b in range(NBLK + 1):
                    if b < NBLK:
                        front(b)
                    if b >= 1:
                        back(b - 1)
                P.barrier()
            with ExitStack() as es:
                A = lambda *a: es.enter_context(nc.sbuf_tensor(self.nm(a[0]), *a[1:]))
                PS = lambda n, sh, dt: es.enter_context(nc.psum_tensor(self.nm(n), sh, dt))
                gb = [A(f"m3_gb{v}", [128, D], F32) for v in range(2)]
                L = self.ln_alloc(es, "m3")
                rows = [[A(f"m3_r{k}_{j}", [128, D], F32) for j in range(4)] for k in range(2)]
                xt = [A(f"m3_xt{k}", [128, D], F32) for k in range(2)]
                acc = [A(f"m3_acc{k}", [128, D], F32) for k in range(2)]
                bdn = A("m3_bdn", [NE, D], F32)
                gT = [A(f"m3_gT{k}", [NE, 128], F32) for k in range(2)]
                dg = [A(f"m3_dg{k}", [128, 4, 128], F32) for k in range(2)]
                ps = PS("m3_ps", [128, 512], F32)
                pgt = PS("m3_pgt", [128, 512], F32)
                pb = [PS(f"m3_pb{k}", [128, 512], F32) for k in range(4)]
                for v in range(2):
                    self.dma('sp', gb[v][:], self.modb[i, 1, v, :, 2 * D:3 * D], r=[], w=[f'gb{v}'])
                self.dma('sp', bdn[:], self.g('moe_b_down')[i], r=[], w=['bdn'])
                self.ln_load(L, i, 1, (ps, 'm3ps'))
                for tt in range(nt):
                    v = 0 if tt < 16 else 1
                    k = tt % 2
                    R, X, AC, GT = rows[k], xt[k], acc[k], gT[k]
                    for j in range(4):
                        P.op('pool', lambda e, R=R, j=j, tt=tt: e.indirect_dma_start(out=R[j][:, :], out_offset=None, in_=self.yslot, in_offset=bass.IndirectOffsetOnAxis(ap=Sk[:, tt * 4 + j:tt * 4 + j + 1], axis=0)),
                             r=[], w=[f'r{k}_{j}'], dma=True)
                    self.dma('act', X[:], src[tt * 128:(tt + 1) * 128, :], r=[f'xs{tt}'], w=[f'x3{k}'])
                    P.op('pe', lambda e, tt=tt: e.transpose(out=pgt[0:NE, 0:128], in_=GD[:, tt, :], identity=self.ident32[:]), r=['ident32'], w=['pgt'])
                    P.op('act', lambda e, GT=GT: e.activation(out=GT[:], in_=pgt[0:NE, 0:128], func=AF.Copy), r=['pgt'], w=[f'gT{k}'])
                    DG = dg[k]
                    for j in range(4):
                        P.op('act', lambda e, DG=DG, j=j, tt=tt: e.activation(out=DG[:, j, :], in_=self.ident32[:], func=AF.Identity, scale=Gk[:, tt * 4 + j:tt * 4 + j + 1]), r=['ident32'], w=[f'dg{k}_{j}'])
                    for q in range(4):
                        P.op('pe', lambda e, DG=DG, R=R, GT=GT, q=q: [e.matmul(pb[q][:], lhsT=DG[:, j, :], rhs=R[j][:, q * 512:(q + 1) * 512], start=(j == 0), stop=False) for j in range(4)]
                             + [e.matmul(pb[q][:], lhsT=GT[:, :], rhs=bdn[:, q * 512:(q + 1) * 512], start=False, stop=True)],
                             r=[f'gT{k}', 'bdn'] + [f'dg{k}_{j}' for j in range(4)] + [f'r{k}_{j}' for j in range(4)], w=[f'pb{q}'])
                        P.op('dve', lambda e, AC=AC, q=q, v=v: e.tensor_tensor(out=AC[:, q * 512:(q + 1) * 512], in0=pb[q][:], in1=gb[v][:, q * 512:(q + 1) * 512], op=ALU.mult), r=[f'pb{q}', f'gb{v}'], w=[f'acc{k}'])
                    P.op('dve', lambda e, AC=AC, X=X: e.scalar_tensor_tensor(out=AC[:], in0=X[:], scalar=float(DN_ALPHA), in1=AC[:], op0=ALU.mult, op1=ALU.add), r=[f'x3{k}'], w=[f'acc{k}'])
                    if dstname == 'y_out':
                        dst = self.y_out[tt * 128:(tt + 1) * 128, :]
                    else:
                        dst = self.xs[tt * 128:(tt + 1) * 128, :]
                    self.ln_tile(L, AC, f'acc{k}', dst, f'xs{tt}')
                P.barrier()


    def stage_proj(self, i, wname, j, srcname, need_ctx):
        nc, P = self.nc, self.P
        src = self.g('x_in') if srcname == 'x_in' else self.xs
        nt = NT if need_ctx else S // 128
        with ExitStack() as es:
            A = lambda *a: es.enter_context(nc.sbuf_tensor(self.nm(a[0]), *a[1:]))
            PS = lambda n, sh, dt: es.enter_context(nc.psum_tensor(self.nm(n), sh, dt))
            wo = A("pj_wo", [128, 16, D], BF16)
            gb = [A(f"pj_gb{v}", [128, D], F32) for v in range(2)]
            L = self.ln_alloc(es, "pj")
            mt = [A(f"pj_mt{k}", [128, 16, 512], BF16) for k in range(2)]
            xt = [A(f"pj_xt{k}", [128, D], F32) for k in range(2)]
            acc = [A(f"pj_acc{k}", [128, D], F32) for k in range(2)]
            po = [PS(f"pj_po{k}", [128, 512], F32) for k in range(4)]
            ps = PS("pj_ps", [128, 512], F32)
            wsrc = self.g(wname)[j].rearrange("(c p) n -> p c n", p=128)
            for h in range(4):
                P.op('pool', lambda e, h=h: e.dma_start(out=wo[:, 4 * h:4 * h + 4, :], in_=wsrc[:, 4 * h:4 * h + 4, :]), r=[], w=[f'wo{h}'], dma=True)
            for v in range(2):
                self.dma('sp', gb[v][:], self.modb[i, 0, v, :, 2 * D:3 * D], r=[], w=[f'gb{v}'])
            self.ln_load(L, i, 0, (ps, 'pjps'))
            msrc = self.mT.rearrange("(c p) t -> p c t", p=128)
            for tt in range(nt):
                v = 0 if tt < 16 else 1
                k = tt % 2
                g4 = tt // 4
                MT = mt[g4 % 2]
                if tt % 4 == 0:
                    n_tok = min(512, nt * 128 - g4 * 512)
                    for h in range(2):
                        self.dma('sp' if h == 0 else 'act', MT[:, 8 * h:8 * h + 8, 0:n_tok], msrc[:, 8 * h:8 * h + 8, g4 * 512:g4 * 512 + n_tok], r=[], w=[f'mt{g4 % 2}_{h}'])
                X, AC = xt[k], acc[k]
                self.dma('act', X[:], src[tt * 128:(tt + 1) * 128, :], r=[f'xs{tt}'], w=[f'pjx{k}'])
                t0 = (tt % 4) * 128
                for q in range(4):
                    P.op('pe', lambda e, MT=MT, q=q, t0=t0: [e.matmul(po[q][:], lhsT=MT[:, c, t0:t0 + 128], rhs=wo[:, c, q * 512:(q + 1) * 512], start=(c == 0), stop=(c == 15)) for c in range(16)],
                         r=[f'mt{g4 % 2}_0', f'mt{g4 % 2}_1'] + [f'wo{h}' for h in range(4)], w=[f'po{q}'])
                    P.op('dve', lambda e, AC=AC, q=q, v=v: e.tensor_tensor(out=AC[:, q * 512:(q + 1) * 512], in0=po[q][:], in1=gb[v][:, q * 512:(q + 1) * 512], op=ALU.mult), r=[f'po{q}', f'gb{v}'], w=[f'pjacc{k}'])
                P.op('dve', lambda e, AC=AC, X=X: e.scalar_tensor_tensor(out=AC[:], in0=X[:], scalar=float(DN_ALPHA), in1=AC[:], op0=ALU.mult, op1=ALU.add), r=[f'pjx{k}'], w=[f'pjacc{k}'])
                self.ln_tile(L, AC, f'pjacc{k}', self.xs[tt * 128:(tt + 1) * 128, :], f'xs{tt}')
            P.barrier()

    def build_aT(self, es, i, src, aT, nt, pfx):
        nc, P = self.nc, self.P
        A = lambda *a: es.enter_context(nc.sbuf_tensor(self.nm(a[0]), *a[1:]))
        PS = lambda n, sh, dt: es.enter_context(nc.psum_tensor(self.nm(n), sh, dt))
        sc1 = [A(f"{pfx}_sc{v}", [128, D], F32) for v in range(2)]
        shb = [A(f"{pfx}_sh{v}", [128, D], F32) for v in range(2)]
        xt = [A(f"{pfx}_xt{k}", [128, D], F32) for k in range(2)]
        a16 = [A(f"{pfx}_a16{k}", [128, D], BF16) for k in range(2)]
        pTb = [PS(f"{pfx}_pTb{k}", [128, 1024], BF16) for k in range(2)]
        for v in range(2):
            self.dma('sp', sc1[v][:], self.modb[i, 0, v, :, D:2 * D], r=[], w=[f'sc1{v}'])
            self.dma('act', shb[v][:], self.modb[i, 0, v, :, 0:D], r=[], w=[f'shb{v}'])
        for tt in range(nt):
            v = 0 if tt < 16 else 1
            k = tt % 2
            X, A16 = xt[k], a16[k]
            self.dma('sp', X[:], src[tt * 128:(tt + 1) * 128, :], r=[f'xs{tt}'], w=[f'atx{k}'])
            P.op('dve', lambda e, X=X, v=v: e.tensor_tensor(out=X[:], in0=X[:], in1=sc1[v][:], op=ALU.mult), r=[f'sc1{v}'], w=[f'atx{k}'])
            P.op('dve', lambda e, X=X, v=v: e.tensor_tensor(out=X[:], in0=X[:], in1=shb[v][:], op=ALU.add), r=[f'shb{v}'], w=[f'atx{k}'])
            P.op('act', lambda e, X=X, A16=A16: e.activation(out=A16[:], in_=X[:], func=AF.Copy), r=[f'atx{k}'], w=[f'ata{k}'])
            for hh in range(2):
                pt = pTb[hh]
                P.op('pe', lambda e, pt=pt, A16=A16, hh=hh: [e.transpose(out=pt[:, jj * 128:(jj + 1) * 128], in_=A16[:, (8 * hh + jj) * 128:(8 * hh + jj + 1) * 128], identity=self.ident16[:]) for jj in range(8)],
                     r=[f'ata{k}', 'ident16'], w=[f'atp{hh}'])
                dst = aT[:, 8 * hh:8 * hh + 8, tt * 128:(tt + 1) * 128]
                srcp = pt[:, :].rearrange("p (a b) -> p a b", a=8)
                if hh == 0:
                    P.op('act', lambda e, dst=dst, srcp=srcp: e.activation(out=dst, in_=srcp, func=AF.Copy), r=[f'atp{hh}'], w=[f'aT{tt}_{hh}'])
                else:
                    P.op('dve', lambda e, dst=dst, srcp=srcp: e.tensor_copy(out=dst, in_=srcp), r=[f'atp{hh}'], w=[f'aT{tt}_{hh}'])
        return [f'aT{tt}_{hh}' for tt in range(nt) for hh in range(2)]

    def stage_lru(self, j, i, srcname):
        nc, P = self.nc, self.P
        src = self.g('x_in') if srcname == 'x_in' else self.xs
        with ExitStack() as es:
            A = lambda *a: es.enter_context(nc.sbuf_tensor(self.nm(a[0]), *a[1:]))
            PS = lambda n, sh, dt: es.enter_context(nc.psum_tensor(self.nm(n), sh, dt))
            aT = A("l1_aT", [128, 16, T], BF16)
            wpan = [A(f"l1_wp{k}", [128, 16, 512], BF16) for k in range(2)]
            ustg = [A(f"l1_us{k}", [128, T], F32) for k in range(2)]
            aT_res = self.build_aT(es, i, src, aT, NT, "l1")
            pu = [PS(f"l1_pu{k}", [128, 512], F32) for k in range(5)]
            wsrc = self.g('lru_w_in')[j].rearrange("(c p) n -> p c n", p=128)
            pieces = [(0, 512), (512, 512), (1024, 512), (1536, 512), (2048, 256)]
            nev = 0
            for pn in range(8):
                WP = wpan[pn % 2]
                for h in range(2):
                    P.op('pool', lambda e, WP=WP, h=h, pn=pn: e.dma_start(out=WP[:, 8 * h:8 * h + 8, :], in_=wsrc[:, 8 * h:8 * h + 8, pn * 512:(pn + 1) * 512]), r=[], w=[f'wp{pn % 2}_{h}'], dma=True)
                for fc in range(4):
                    F = pn * 4 + fc
                    US = ustg[F % 2]
                    for pc, (p0, n_) in enumerate(pieces):
                        P.op('pe', lambda e, WP=WP, fc=fc, pc=pc, p0=p0, n_=n_: [e.matmul(pu[pc][:, 0:n_], lhsT=WP[:, c, fc * 128:(fc + 1) * 128], rhs=aT[:, c, p0:p0 + n_], start=(c == 0), stop=(c == 15)) for c in range(16)],
                             r=[f'wp{pn % 2}_0', f'wp{pn % 2}_1'] + (aT_res if (pn == 0 and fc == 0) else []), w=[f'pu{pc}'])
                        if nev % 2 == 0:
                            P.op('act', lambda e, US=US, pc=pc, p0=p0, n_=n_: e.activation(out=US[:, p0:p0 + n_], in_=pu[pc][:, 0:n_], func=AF.Copy), r=[f'pu{pc}'], w=[f'us{F % 2}'])
                        else:
                            P.op('dve', lambda e, US=US, pc=pc, p0=p0, n_=n_: e.tensor_copy(out=US[:, p0:p0 + n_], in_=pu[pc][:, 0:n_]), r=[f'pu{pc}'], w=[f'us{F % 2}'])
                        nev += 1
                    self.dma('sp', self.uT[F * 128:(F + 1) * 128, :], US[:], r=[f'us{F % 2}'], w=[])
            P.barrier()
        W = 2309
        LAT0, LAT1, CTX0, CTX1 = 1, 2049, 2051, 2307
        NW = CTX1 - LAT0
        with ExitStack() as es:
            A = lambda *a: es.enter_context(nc.sbuf_tensor(self.nm(a[0]), *a[1:]))
            PS = lambda n, sh, dt: es.enter_context(nc.psum_tensor(self.nm(n), sh, dt))
            prow = [A(f"l2_prow{k}", [128, 128], F32) for k in range(2)]
            pc_ = A("l2_pc", [128, 256], F32)
            cexp = A("l2_cexp", [128, 64], F32)
            rp = [A(f"l2_rp{k}", [128, 2, W], F32) for k in range(2)]
            ug = [A("l2_ug", [128, 2, T], F32)] * 2
            rc = A("l2_rc", [128, 2, W], F32)
            rc16 = A("l2_rc16", [128, 2, W], BF16)
            yacc = A("l2_yacc", [128, 2, W], F32)
            ras = [A(f"l2_ra{z}", [128, W], F32) for z in range(2)]
            ias = [A(f"l2_ia{z}", [128, W], F32) for z in range(2)]
            aas = [A(f"l2_aa{z}", [128, W], F32) for z in range(2)]
            s2s = [A(f"l2_s2{z}", [128, W], F32) for z in range(2)]
            hbs = [A("l2_hb", [128, W], F32)] * 2
            mo = [A(f"l2_mo{k}", [128, T], BF16) for k in range(2)]
            gw = [A(f"l2_gw{k}", [128, 4, 2, 256], BF16) for k in range(2)]
            pg = [PS(f"l2_pg{k}", [128, 512], F32) for k in range(5)]
            ptp = PS("l2_ptp", [128, 512], F32)
            self.dma('sp', prow[0][0:64, :], self.g('lru_conv_w')[j].rearrange("k (c p) -> (k c) p", p=128), r=[], w=['prow0'])
            self.dma('sp', prow[0][64:80, :], self.g('lru_conv_b')[j:j + 1, :].rearrange("o (c p) -> (o c) p", p=128), r=[], w=['prow0'])
            P.op('pool', lambda e: e.memset(prow[0][64:128, :], 0.0), r=[], w=['prow0z'])
            self.dma('sp', prow[1][0:64, :], self.g('lru_gate_b')[j].rearrange("d g (c p) -> (d g c) p", p=128), r=[], w=['prow1'])
            self.dma('sp', prow[1][64:96, :], self.g('lru_lambda')[j].rearrange("d (c p) -> (d c) p", p=128), r=[], w=['prow1'])
            P.op('pool', lambda e: e.memset(prow[1][64:128, :], 0.0), r=[], w=['prow1z'])
            P.barrier()
            self.dma('sp', prow[0][64:80, :], self.g('lru_conv_b')[j:j + 1, :].rearrange("o (c p) -> (o c) p", p=128), r=[], w=['prow0'])
            self.dma('sp', prow[1][64:96, :], self.g('lru_lambda')[j].rearrange("d (c p) -> (d c) p", p=128), r=[], w=['prow1'])
            for k in range(2):
                P.op('pe', lambda e, k=k: e.transpose(out=ptp[:, k * 128:(k + 1) * 128], in_=prow[k][:], identity=self.ident32[:]), r=[f'prow{k}', 'ident32'], w=['ptp'])
            P.op('act', lambda e: e.activation(out=pc_[:], in_=ptp[:, 0:256], func=AF.Copy), r=['ptp'], w=['pc'])
            P.op('act', lambda e: e.activation(out=cexp[:, 0:32], in_=pc_[:, 192:224], func=AF.Exp, scale=-1.0), r=['pc'], w=['cexp'])
            P.op('act', lambda e: e.activation(out=cexp[:, 0:32], in_=cexp[:, 0:32], func=AF.Ln, bias=1.0, scale=1.0), r=[], w=['cexp'])
            P.op('dve', lambda e: e.tensor_scalar(out=cexp[:, 32:64], in0=cexp[:, 0:32], scalar1=-16.0, scalar2=None, op0=ALU.mult), r=[], w=['cexp'])
            P.op('dve', lambda e: e.tensor_scalar(out=cexp[:, 0:32], in0=cexp[:, 0:32], scalar1=-8.0, scalar2=None, op0=ALU.mult), r=[], w=['cexp'])
            for k in range(2):
                P.op('pool', lambda e, k=k: e.memset(rp[k][:], 0.0), r=[], w=[f'rp{k}'])
            P.op('pool', lambda e: e.memset(yacc[:], 0.0), r=[], w=['yacc'])
            P.op('pool', lambda e: e.memset(hbs[0][:], 0.0), r=[], w=['hb'])
            itc = 0
            pieces = [(LAT0 + q * 512, min(512, CTX1 - (LAT0 + q * 512))) for q in range(5)]
            gwsrc = self.g('lru_gate_w')[j]
            for n in range(8):
                b2 = n % 2
                RP, UG, GW = rp[b2], ug[b2], gw[b2]
                for k in range(2):
                    row0 = 2048 + n * 256 + k * 128
                    self.dma('sp', RP[:, k, LAT0:LAT1], self.uT[row0:row0 + 128, 0:S], r=[], w=[f'rp{b2}'])
                    self.dma('sp', RP[:, k, CTX0:CTX1], self.uT[row0:row0 + 128, S:T], r=[], w=[f'rp{b2}'])
                    self.dma('act', UG[:, k, :], self.uT[n * 256 + k * 128:n * 256 + (k + 1) * 128, :], r=[], w=[f'ug_{k}'])
                for d_ in range(2):
                    for g_ in range(2):
                        P.op('pool', lambda e, GW=GW, d_=d_, g_=g_, n=n: e.dma_start(out=GW[:, d_ * 2 + g_, :, :], in_=gwsrc[d_, g_, n].rearrange("(kc p) o -> p kc o", p=128)), r=[], w=[f'gw{b2}'], dma=True)
                for k in range(2):
                    ch = n * 2 + k
                    P.op('act', lambda e, UG=UG, k=k: e.activation(out=UG[:, k, :], in_=UG[:, k, :], func=AF.Gelu_apprx_tanh), r=[], w=[f'ug_{k}'])
                    P.op('dve', lambda e, RP=RP, k=k, ch=ch: e.tensor_scalar(out=rc[:, k, LAT0:CTX1], in0=RP[:, k, 0:NW], scalar1=pc_[:, ch:ch + 1], scalar2=pc_[:, 64 + ch:65 + ch], op0=ALU.mult, op1=ALU.add), r=[f'rp{b2}', 'pc'], w=[f'rc{k}'])
                    for t_ in range(1, 4):
                        P.op('dve', lambda e, RP=RP, k=k, ch=ch, t_=t_: e.scalar_tensor_tensor(out=rc[:, k, LAT0:CTX1], in0=RP[:, k, t_:t_ + NW], scalar=pc_[:, t_ * 16 + ch:t_ * 16 + ch + 1], in1=rc[:, k, LAT0:CTX1], op0=ALU.mult, op1=ALU.add), r=[f'rp{b2}'], w=[f'rc{k}'])
                    P.op('act', lambda e, k=k: e.activation(out=rc16[:, k, LAT0:CTX1], in_=rc[:, k, LAT0:CTX1], func=AF.Copy), r=[f'rc{k}'], w=[f'rc16_{k}'])
                for d_ in range(2):
                    for jc in range(2):
                        ch = n * 2 + jc
                        z = itc % 2
                        itc += 1
                        ra, ia, aa, s2, hb = ras[z], ias[z], aas[z], s2s[z], hbs[z]
                        for g_, dstt, dres in ((0, ra, f'ra{z}'), (1, ia, f'ia{z}')):
                            bcol = pc_[:, 128 + (d_ * 2 + g_) * 16 + ch:128 + (d_ * 2 + g_) * 16 + ch + 1]
                            for pc, (p0, n_) in enumerate(pieces):
                                P.op('pe', lambda e, GW=GW, d_=d_, g_=g_, jc=jc, pc=pc, p0=p0, n_=n_: [e.matmul(pg[pc][:, 0:n_], lhsT=GW[:, d_ * 2 + g_, kc, jc * 128:(jc + 1) * 128], rhs=rc16[:, kc, p0:p0 + n_], start=(kc == 0), stop=(kc == 1)) for kc in range(2)],
                                     r=[f'gw{b2}', 'rc16_0', 'rc16_1'], w=[f'pg{pc}'])
                                P.op('act', lambda e, dstt=dstt, pc=pc, p0=p0, n_=n_, bcol=bcol: e.activation(out=dstt[:, p0:p0 + n_], in_=pg[pc][:, 0:n_], func=AF.Sigmoid, bias=bcol, scale=1.0), r=[f'pg{pc}', 'pc'], w=[dres])
                        c1 = cexp[:, d_ * 16 + ch:d_ * 16 + ch + 1]
                        c2 = cexp[:, 32 + d_ * 16 + ch:32 + d_ * 16 + ch + 1]
                        P.op('act', lambda e, c1=c1, aa=aa, ra=ra: e.activation(out=aa[:, LAT0:CTX1], in_=ra[:, LAT0:CTX1], func=AF.Exp, scale=c1), r=[f'ra{z}', 'cexp'], w=[f'aa{z}'])
                        P.op('act', lambda e, c2=c2, s2=s2, ra=ra: e.activation(out=s2[:, LAT0:CTX1], in_=ra[:, LAT0:CTX1], func=AF.Exp, scale=c2), r=[f'ra{z}', 'cexp'], w=[f's2{z}'])
                        P.op('act', lambda e, s2=s2: e.activation(out=s2[:, LAT0:CTX1], in_=s2[:, LAT0:CTX1], func=AF.Sqrt, scale=-1.0, bias=1.0), r=[], w=[f's2{z}'])
                        P.op('dve', lambda e, jc=jc, ia=ia: e.tensor_tensor(out=ia[:, LAT0:CTX1], in0=ia[:, LAT0:CTX1], in1=rc[:, jc, LAT0:CTX1], op=ALU.mult), r=[f'rc{jc}'], w=[f'ia{z}'])
                        P.op('dve', lambda e, ia=ia, s2=s2: e.tensor_tensor(out=ia[:, LAT0:CTX1], in0=ia[:, LAT0:CTX1], in1=s2[:, LAT0:CTX1], op=ALU.mult), r=[f's2{z}'], w=[f'ia{z}'])
                        if d_ == 0:
                            P.op('dve', lambda e, jc=jc, aa=aa, ia=ia: e.tensor_tensor_scan(out=yacc[:, jc, CTX0:CTX1], data0=aa[:, CTX0:CTX1], data1=ia[:, CTX0:CTX1], initial=0.0, op0=ALU.mult, op1=ALU.add), r=[f'aa{z}', f'ia{z}'], w=[f'yacc{jc}'])
                            P.op('dve', lambda e, jc=jc, aa=aa, ia=ia: e.tensor_tensor_scan(out=yacc[:, jc, LAT0:LAT1], data0=aa[:, LAT0:LAT1], data1=ia[:, LAT0:LAT1], initial=yacc[:, jc, CTX1 - 1:CTX1], op0=ALU.mult, op1=ALU.add), r=[f'aa{z}', f'ia{z}'], w=[f'yacc{jc}'])
                        else:
                            P.op('dve', lambda e, hb=hb, aa=aa, ia=ia: e.tensor_tensor_scan(out=rev_ap(hb[:, CTX0:CTX1]), data0=rev_ap(aa[:, CTX0:CTX1]), data1=rev_ap(ia[:, CTX0:CTX1]), initial=0.0, op0=ALU.mult, op1=ALU.add), r=[f'aa{z}', f'ia{z}'], w=['hb'])
                            P.op('dve', lambda e, hb=hb, aa=aa, ia=ia: e.tensor_tensor_scan(out=rev_ap(hb[:, LAT0:LAT1]), data0=rev_ap(aa[:, LAT0:LAT1]), data1=rev_ap(ia[:, LAT0:LAT1]), initial=hb[:, CTX0:CTX0 + 1], op0=ALU.mult, op1=ALU.add), r=[f'aa{z}', f'ia{z}'], w=['hb'])
                            P.op('dve', lambda e, jc=jc, hb=hb: e.tensor_tensor(out=yacc[:, jc, LAT0:CTX1], in0=yacc[:, jc, LAT0:CTX1], in1=hb[:, LAT0:CTX1], op=ALU.add), r=['hb'], w=[f'yacc{jc}'])
                for k in range(2):
                    MO = mo[k]
                    P.op('pool', lambda e, MO=MO, UG=UG, k=k: e.tensor_tensor(out=MO[:, 0:S], in0=yacc[:, k, LAT0:LAT1], in1=UG[:, k, 0:S], op=ALU.mult), r=[f'yacc{k}', f'ug_{k}'], w=[f'mo{k}'])
                    P.op('pool', lambda e, MO=MO, UG=UG, k=k: e.tensor_tensor(out=MO[:, S:T], in0=yacc[:, k, CTX0:CTX1], in1=UG[:, k, S:T], op=ALU.mult), r=[f'yacc{k}', f'ug_{k}'], w=[f'mo{k}'])
                    self.dma('sp', self.mT[(n * 2 + k) * 128:(n * 2 + k + 1) * 128, :], MO[:], r=[f'mo{k}'], w=[])
            P.barrier()


    def stage_att(self, j, i, srcname, need_ctx):
        nc, P = self.nc, self.P
        src = self.g('x_in') if srcname == 'x_in' else self.xs
        nq = NT if need_ctx else S // 128
        with ExitStack() as es0:
            A0 = lambda *a: es0.enter_context(nc.sbuf_tensor(self.nm(a[0]), *a[1:]))
            kTe = A0("at_kTe", [128, 4, T], BF16)
            kTo = A0("at_kTo", [128, 4, T], BF16)
            v1 = A0("at_v1", [128, NT, 4, 68], BF16)
            with ExitStack() as es:
                A = lambda *a: es.enter_context(nc.sbuf_tensor(self.nm(a[0]), *a[1:]))
                PS = lambda n, sh, dt: es.enter_context(nc.psum_tensor(self.nm(n), sh, dt))
                wq = A("t1_wq", [128, 16, 2560], BF16)
                sc1 = A("t1_sc", [128, D], F32)
                shb = A("t1_sh", [128, D], F32)
                xt = [A(f"t1_xt{k}", [128, D], F32) for k in range(2)]
                a16 = [A("t1_a16", [128, D], BF16)] * 2
                aTt = [A("t1_aT", [128, 16, 128], BF16)] * 2
                q32s = [A(f"t1_q32{z}", [128, D], F32) for z in range(2)]
                k32s = [A("t1_k32", [128, 256], F32)] * 2
                q16s = [A(f"t1_q16{z}", [128, D], BF16) for z in range(2)]
                k16s = [A(f"t1_k16{z}", [128, 256], BF16) for z in range(2)]
                kd = A("t1_kd", [128, 2, 4, 2, 64], BF16)
                tA = A("t1_tA", [128, 1024], F32)
                tB = A("t1_tB", [128, 1024], F32)
                qst = [A("t1_qst", [128, 16, 128], BF16)] * 2
                rts = [A(f"t1_rt{z}", [128, 64], F32) for z in range(2)]
                pTb = [PS(f"t1_pTb{k}", [128, 1024], BF16) for k in range(2)]
                po = [PS(f"t1_po{k}", [128, 512], F32) for k in range(5)]
                wsrc = self.g('attn_w_qkv')[j].rearrange("(c p) n -> p c n", p=128)
                for h in range(4):
                    for a_ in range(2):
                        P.op('pool', lambda e, h=h, a_=a_: e.dma_start(out=wq[:, 4 * h:4 * h + 4, a_ * 1280:(a_ + 1) * 1280], in_=wsrc[:, 4 * h:4 * h + 4, a_ * 1280:(a_ + 1) * 1280]), r=[], w=[f'wq{h}_{a_}'], dma=True)
                P.op('dve', lambda e: e.memset(v1[:].rearrange("p a b c -> p (a b c)"), 1.0), r=[], w=['v1'])
                P.op('dve', lambda e: e.memset(kd[:].rearrange("p a b c d -> p (a b c d)"), 0.0), r=[], w=['kd0', 'kd1'])
                for tt in range(NT):
                    v = 0 if tt < 16 else 1
                    k = tt % 2
                    if tt == 0 or tt == 16:
                        self.dma('sp', sc1[:], self.modb[i, 0, v, :, D:2 * D], r=[], w=['sc1'])
                        self.dma('act', shb[:], self.modb[i, 0, v, :, 0:D], r=[], w=['shb'])
                    X, A16, AT, QS = xt[k], a16[k], aTt[k], qst[k]
                    q32, k32, q16, k16 = q32s[k], k32s[k], q16s[k], k16s[k]
                    self.dma('sp', X[:], src[tt * 128:(tt + 1) * 128, :], r=[f'xs{tt}'], w=[f'x{k}'])
                    P.op('dve', lambda e, X=X: e.tensor_tensor(out=X[:], in0=X[:], in1=sc1[:], op=ALU.mult), r=['sc1'], w=[f'x{k}'])
                    P.op('dve', lambda e, X=X: e.tensor_tensor(out=X[:], in0=X[:], in1=shb[:], op=ALU.add), r=['shb'], w=[f'x{k}'])
                    P.op('act', lambda e, X=X, A16=A16: e.activation(out=A16[:], in_=X[:], func=AF.Copy), r=[f'x{k}'], w=['a16s'])
                    for hh in range(2):
                        pt = pTb[hh]
                        P.op('pe', lambda e, pt=pt, A16=A16, hh=hh: [e.transpose(out=pt[:, jj * 128:(jj + 1) * 128], in_=A16[:, (8 * hh + jj) * 128:(8 * hh + jj + 1) * 128], identity=self.ident16[:]) for jj in range(8)],
                             r=['a16s', 'ident16'], w=[f'pTb{hh}'])
                        dst = AT[:, 8 * hh:8 * hh + 8, :].rearrange("p a b -> p (a b)")
                        if hh == 0:
                            P.op('act', lambda e, dst=dst, pt=pt: e.activation(out=dst, in_=pt[:], func=AF.Copy), r=[f'pTb{hh}'], w=[f'aTs_{hh}'])
                        else:
                            P.op('dve', lambda e, dst=dst, pt=pt: e.tensor_copy(out=dst, in_=pt[:]), r=[f'pTb{hh}'], w=[f'aTs_{hh}'])
                    need_q = tt < nq
                    for q in range(5):
                        if q < 4 and not need_q:
                            continue
                        P.op('pe', lambda e, AT=AT, q=q: [e.matmul(po[q][:], lhsT=AT[:, c, :], rhs=wq[:, c, q * 512:(q + 1) * 512], start=(c == 0), stop=(c == 15)) for c in range(16)],
                             r=['aTs_0', 'aTs_1'] + [f'wq{h}_{a_}' for h in range(4) for a_ in range(2)], w=[f'po{q}'])
                        if q < 4:
                            P.op('act', lambda e, q=q, q32=q32: e.activation(out=q32[:, q * 512:(q + 1) * 512], in_=po[q][:], func=AF.Copy, scale=0.125), r=[f'po{q}'], w=[f'q32{k}_{q}'])
                        else:
                            P.op('dve', lambda e, k32=k32: e.tensor_copy(out=k32[:], in_=po[4][:, 0:256]), r=['po4'], w=['k32'])
                            if 'skipD' not in self.dbg:
                                P.op('dve', lambda e, tt=tt: e.tensor_copy(out=v1[:, tt, :, 0:64], in_=po[4][:, 256:512].rearrange("p (g d) -> p g d", g=4)), r=['po4'], w=['v1'])
                    if tt < 16 and 'skipA' not in self.dbg:
                        rtk = rts[k]
                        self.dma('act', rtk[:], self.g('rope_t')[tt * 128:(tt + 1) * 128, :], r=[], w=[f'rt{k}'])

                        def rope(x32, x16, nh, xres, ores, tt=tt, rtk=rtk, k=k):
                            cos_ = sub_ap(rtk[:, 0:32], [(0, nh), (16, 2), (1, 16)])
                            sin_ = sub_ap(rtk[:, 32:64], [(0, nh), (16, 2), (1, 16)])
                            x1 = sub_ap(x32[:, 0:1], [(64, nh), (32, 2), (1, 16)])
                            x2 = sub_ap(x32[:, 16:17], [(64, nh), (32, 2), (1, 16)])
                            o1 = sub_ap(x16[:, 0:1], [(64, nh), (32, 2), (1, 16)])
                            o2 = sub_ap(x16[:, 16:17], [(64, nh), (32, 2), (1, 16)])
                            ta = sub_ap(tA[:, 0:1], [(32, nh), (16, 2), (1, 16)])
                            tb = sub_ap(tB[:, 0:1], [(32, nh), (16, 2), (1, 16)])
                            P.op('dve', lambda e: e.tensor_tensor(out=ta, in0=x1, in1=cos_, op=ALU.mult), r=xres + [f'rt{k}'], w=['tA'])
                            P.op('dve', lambda e: e.tensor_tensor(out=tb, in0=x2, in1=sin_, op=ALU.mult), r=xres + [f'rt{k}'], w=['tB'])
                            P.op('dve', lambda e: e.tensor_tensor(out=o1, in0=ta, in1=tb, op=ALU.subtract), r=['tA', 'tB'], w=[ores + 'a'])
                            P.op('dve', lambda e: e.tensor_tensor(out=ta, in0=x1, in1=sin_, op=ALU.mult), r=xres + [f'rt{k}'], w=['tA'])
                            P.op('dve', lambda e: e.tensor_tensor(out=tb, in0=x2, in1=cos_, op=ALU.mult), r=xres + [f'rt{k}'], w=['tB'])
                            P.op('dve', lambda e: e.tensor_tensor(out=o2, in0=ta, in1=tb, op=ALU.add), r=['tA', 'tB'], w=[ores + 'b'])
                        if need_q:
                            rope(q32, q16, 32, [f'q32{k}_{q}' for q in range(4)], f'q16{k}')
                        rope(k32, k16, 4, ['k32'], f'k16{k}')
                    elif tt >= 16:
                        if need_q:
                            P.op('act', lambda e, q16=q16, q32=q32: e.activation(out=q16[:], in_=q32[:], func=AF.Copy), r=[f'q32{k}_{q}' for q in range(4)], w=[f'q16{k}a', f'q16{k}b'])
                        P.op('dve', lambda e, k16=k16, k32=k32: e.tensor_copy(out=k16[:], in_=k32[:]), r=['k32'], w=[f'k16{k}a', f'k16{k}b'])
                    for dd in range(2 if 'skipB' not in self.dbg else 0):
                        P.op('dve', lambda e, dd=dd, k16=k16: e.tensor_copy(out=kd[:, dd, :, dd, :], in_=k16[:, :].rearrange("p (g d) -> p g d", g=4)), r=[f'k16{k}a', f'k16{k}b'], w=[f'kd{dd}'])
                    if 'skipB' not in self.dbg:
                        P.op('pe', lambda e: [e.transpose(out=pTb[0][:, (dd * 4 + g) * 128:(dd * 4 + g + 1) * 128], in_=kd[:, dd, g, :, :].rearrange("p a d -> p (a d)"), identity=self.ident16[:]) for dd in range(2) for g in range(4)],
                             r=['kd0', 'kd1', 'ident16'], w=['pTb0'])
                        P.op('dve', lambda e, tt=tt: e.tensor_copy(out=kTe[:, :, tt * 128:(tt + 1) * 128], in_=pTb[0][:, 0:512].rearrange("p (g t) -> p g t", g=4)), r=['pTb0'], w=['kTe'])
                        P.op('dve', lambda e, tt=tt: e.tensor_copy(out=kTo[:, :, tt * 128:(tt + 1) * 128], in_=pTb[0][:, 512:1024].rearrange("p (g t) -> p g t", g=4)), r=['pTb0'], w=['kTo'])
                    if need_q and 'skipC' not in self.dbg:
                        for hh in range(2):
                            pt = pTb[hh]
                            P.op('pe', lambda e, pt=pt, hh=hh, q16=q16: [e.transpose(out=pt[:, jj * 128:(jj + 1) * 128], in_=q16[:, (8 * hh + jj) * 128:(8 * hh + jj + 1) * 128], identity=self.ident16[:]) for jj in range(8)],
                                 r=[f'q16{k}a', f'q16{k}b', 'ident16'], w=[f'pTb{hh}'])
                            dst = QS[:, 8 * hh:8 * hh + 8, :].rearrange("p a b -> p (a b)")
                            if hh == 0:
                                P.op('act', lambda e, dst=dst, pt=pt: e.activation(out=dst, in_=pt[:], func=AF.Copy), r=[f'pTb{hh}'], w=['qst'])
                            else:
                                P.op('dve', lambda e, dst=dst, pt=pt: e.tensor_copy(out=dst, in_=pt[:]), r=[f'pTb{hh}'], w=['qst'])
                        self.dma('sp', self.qTd[tt], QS[:], r=['qst'], w=[])
                P.barrier()
            if 't1only' in self.dbg:
                self.dbg_k = self.scratch_out("dbg_kTd", [128, 4, T], BF16)
                kTd = kTe
                self.dbg_v = self.scratch_out("dbg_v1", [128, NT, 4, 68], BF16)
                self.dma('sp', self.dbg_k, kTd[:], r=[], w=[])
                self.dma('sp', self.dbg_v, v1[:], r=[], w=[])
                P.barrier()
                return
            with ExitStack() as es:
                A = lambda *a: es.enter_context(nc.sbuf_tensor(self.nm(a[0]), *a[1:]))
                PS = lambda n, sh, dt: es.enter_context(nc.psum_tensor(self.nm(n), sh, dt))
                qT = [A(f"t2_qT{k}", [128, 16, 128], BF16) for k in range(2)]
                PT = [[A(f"t2_PT{k}_{m}", [128, 1024], BF16) for m in range(5)] for k in range(2)]
                o16 = [A(f"t2_o16{k}", [128, D], BF16) for k in range(2)]
                oT = [A(f"t2_oT{k}", [128, 16, 128], BF16) for k in range(2)]
                srow = A("t2_srow", [1, 32], F32)
                esink = A("t2_esink", [128, 32], F32)
                m32 = A("t2_m32", [128, 2, 128], F32)
                m16 = A("t2_m16", [128, 2, 128], BF16)
                den = [A(f"t2_den{k}", [128, 16], F32) for k in range(2)]
                psS = [PS(f"t2_psS{k}", [128, 1024], F32) for k in range(2)]
                pso = PS("t2_pso", [128, 8, 128], F32)
                pTo = PS("t2_pTo", [128, 1024], BF16)
                self.dma('sp', srow[:], self.g('attn_sinks')[j:j + 1, :], r=[], w=['t2_row'])
                self.bcast_rows(None, (psS[0], 'psS0'), srow, esink, 't2')
                P.op('act', lambda e: e.activation(out=esink[:], in_=esink[:], func=AF.Exp), r=[], w=['t2'])
                self.dma('sp', m32[:], self.g('tri')[1:3].rearrange("m k q -> k m q"), r=[], w=['m32'])
                P.op('dve', lambda e: e.tensor_copy(out=m16[:], in_=m32[:]), r=['m32'], w=['m16'])
                ns = 0
                for qb in range(nq):
                    kq = qb % 2
                    QT = qT[kq]
                    self.dma('sp', QT[:], self.qTd[qb], r=[], w=[f'qT{kq}'])
                    if qb < 16:
                        kbs = [(kb, (1 if kb == qb - 1 else (2 if kb == qb + 1 else 0))) for kb in (qb - 1, qb, qb + 1) if 0 <= kb < 16] + [(16, 0), (17, 0)]
                    else:
                        kbs = [(16, 0), (17, 0)]
                    O16 = o16[kq]
                    for g in range(4):
                        PTs = PT[g % 2]
                        for m, (kb, mk) in enumerate(kbs):
                            pS = psS[ns % 2]
                            pSr = f'psS{ns % 2}'
                            ns += 1
                            P.op('pe', lambda e, pS=pS, g=g, kb=kb, QT=QT: [e.matmul(pS[:, (2 * cc + hh) * 128:(2 * cc + hh + 1) * 128], lhsT=(kTe if hh == 0 else kTo)[:, g, kb * 128:(kb + 1) * 128], rhs=QT[:, 4 * g + cc, :], start=True, stop=True) for cc in range(4) for hh in range(2)],
                                 r=[f'qT{kq}'], w=[pSr])
                            P.op('act', lambda e, pS=pS, PTs=PTs, m=m: [e.activation(out=PTs[m][:, 512 * z:512 * (z + 1)], in_=pS[:, 512 * z:512 * (z + 1)], func=AF.Exp) for z in range(2)], r=[pSr], w=[f'PT{g % 2}_{m}'])
                            if mk and 'skipE' not in self.dbg:
                                P.op('dve', lambda e, PTs=PTs, m=m, mk=mk: e.tensor_tensor(out=PTs[m][:, :].rearrange("p (h q) -> p h q", h=8), in0=PTs[m][:, :].rearrange("p (h q) -> p h q", h=8), in1=sub_ap(m16[:, mk - 1, :], [(0, 8), (1, 128)]), op=ALU.mult), r=['m16'], w=[f'PT{g % 2}_{m}'])
                        nk = len(kbs)
                        if 'skipF' in self.dbg:
                            continue
                        P.op('pe', lambda e, PTs=PTs, kbs=kbs, g=g, nk=nk: [e.matmul(pso[:, hl, 0:65], lhsT=PTs[m][:, hl * 128:(hl + 1) * 128], rhs=v1[:, kb, g, 0:65], start=(m == 0), stop=(m == nk - 1)) for hl in range(8) for m, (kb, mk) in enumerate(kbs)],
                             r=[f'PT{g % 2}_{m}' for m in range(nk)], w=['pso'])
                        DN = den[g % 2]
                        P.op('dve', lambda e, DN=DN, g=g: e.tensor_tensor(out=DN[:, 0:8], in0=pso[:, :, 64], in1=esink[:, 8 * g:8 * g + 8], op=ALU.add), r=['pso', 't2'], w=[f'den{g % 2}'])
                        P.op('dve', lambda e, DN=DN: e.reciprocal(out=DN[:, 8:16], in_=DN[:, 0:8]), r=[], w=[f'den{g % 2}'])
                        P.op('dve', lambda e, DN=DN, O16=O16, g=g: e.tensor_tensor(out=O16[:, 512 * g:512 * (g + 1)].rearrange("p (h d) -> p h d", h=8), in0=pso[:, :, 0:64], in1=sub_ap(DN[:, 8:16], [(1, 8), (0, 64)]), op=ALU.mult), r=['pso', f'den{g % 2}'], w=[f'o16{kq}_{g}'])
                    if 'skipF' in self.dbg or 'skipG' in self.dbg:
                        continue
                    OT = oT[kq]
                    for hh in range(2):
                        P.op('pe', lambda e, O16=O16, hh=hh: [e.transpose(out=pTo[:, jj * 128:(jj + 1) * 128], in_=O16[:, (8 * hh + jj) * 128:(8 * hh + jj + 1) * 128], identity=self.ident16[:]) for jj in range(8)],
                             r=[f'o16{kq}_{g}' for g in range(4)] + ['ident16'], w=['t2pTb'])
                        dst = OT[:, 8 * hh:8 * hh + 8, :].rearrange("p a b -> p (a b)")
                        P.op('act', lambda e, dst=dst: e.activation(out=dst, in_=pTo[:], func=AF.Copy), r=['t2pTb'], w=[f'oT{kq}'])
                    self.dma('sp', self.mT.rearrange("(c p) t -> p c t", p=128)[:, :, qb * 128:(qb + 1) * 128], OT[:], r=[f'oT{kq}'], w=[])
                P.barrier()


def build(plan, dbg=()):
    B = Builder(plan, dbg)
    B.declare_io()
    B.consts()
    for st in plan:
        getattr(B, 'stage_' + st[0])(*st[1:])
    B.P.finish()
    return B


def host_consts():
    pos = np.arange(S)
    rowp = (pos // 64).astype(np.float32)
    colp = (pos % 64).astype(np.float32)
    freqs = (10000.0 ** (-np.arange(0, 32, 2, dtype=np.float32) / 32)).astype(np.float32)
    ar = rowp[:, None] * freqs[None, :]
    ac = colp[:, None] * freqs[None, :]
    rope = np.concatenate([np.cos(ar), np.cos(ac), np.sin(ar), np.sin(ac)], axis=1).astype(np.float32)
    kk = np.arange(128)[:, None]
    qq = np.arange(128)[None, :]
    tri = np.stack([(kk < qq), (qq <= kk), (kk <= qq)]).astype(np.float32)
    iota = np.tile(np.arange(512, dtype=np.float32)[None, :], (128, 1))
    return {"ident": np.eye(128, dtype=np.float32), "rope_t": rope, "tri": tri, "iota": iota,
            "pcol": np.arange(128, dtype=np.float32).reshape(128, 1)}


WNAMES = ['ada_w', 'ada_b', 'ln_g', 'ln_b', 'lru_w_in', 'lru_conv_w', 'lru_conv_b', 'lru_gate_w',
          'lru_gate_b', 'lru_lambda', 'lru_w_out', 'attn_w_qkv', 'attn_sinks', 'attn_w_o', 'router_w',
          'router_b', 'moe_w_gu', 'moe_b_gu', 'moe_w_down', 'moe_b_down']


def make_in_maps(inputs, cores, x_override=None):
    hc = host_consts()
    maps = []
    for b in cores:
        m = {k: np.ascontiguousarray(inputs[k]) for k in WNAMES if k in inputs}
        m.update(hc)
        if x_override is not None:
            m["x_in"] = x_override[b]
        else:
            m["x_in"] = np.ascontiguousarray(np.concatenate([inputs['x'][b], inputs['ctx'][b]], axis=0))
        m["cvec"] = np.ascontiguousarray(np.stack([inputs['c'][b], inputs['c_ctx']]).reshape(32, 128))
        maps.append(m)
    return maps


def full_plan():
    plan = [('ada', [0, 1, 2, 3])]
    for i in range(DEPTH):
        need_ctx = i < DEPTH - 1
        srcname = 'x_in' if i == 0 else 'xs'
        j = i // 2
        if i % 2 == 0:
            plan.append(('lru', j, i, srcname))
            plan.append(('proj', i, 'lru_w_out', j, srcname, True))
        else:
            plan.append(('att', j, i, srcname, need_ctx))
            plan.append(('proj', i, 'attn_w_o', j, srcname, need_ctx))
        plan.append(('moe', i, 'xs', 'y_out' if i == DEPTH - 1 else 'xs', need_ctx))
    return plan


_CACHE = {}


def kernel(**inputs):
    inputs = {k: np.asarray(v) for k, v in inputs.items()}
    nb = inputs['x'].shape[0]
    if 'B' not in _CACHE:
        _CACHE['B'] = build(full_plan())
    B = _CACHE['B']
    need = list(B.din.keys())
    maps = make_in_maps(inputs, list(range(nb)))
    maps = [{k: np.ascontiguousarray(m[k], dtype=np.float32) for k in need} for m in maps]
    res = run_bass_kernel_spmd(B.nc, maps, core_ids=list(range(nb)))
    out = np.stack([np.asarray(r['y_out'], dtype=np.float32) for r in res.results], axis=0)
    return out
```

```python
import re
import numpy as np
from contextlib import ExitStack
import concourse.bass as bass
import concourse.mybir as mybir
from concourse.bass_utils import run_bass_kernel_spmd

F32 = mybir.dt.float32
BF16 = mybir.dt.bfloat16
U32 = mybir.dt.uint32
I32 = mybir.dt.int32
AF = mybir.ActivationFunctionType
ALU = mybir.AluOpType
AX = mybir.AxisListType

D = 2048
S = 2048
C = 256
T = S + C
NT = T // 128
DEPTH = 4
NE = 32
DE = 768
CAP = 384
DN_ALPHA = (2.0 * DEPTH) ** 0.25
LN_EPS = 1e-5
ENGS = ['pe', 'act', 'dve', 'pool', 'sp']


class Prog:
    def __init__(self, nc, n_dma_sems=40):
        self.nc = nc
        self.ops = {e: [] for e in ENGS}
        self.sem = {e: nc.alloc_semaphore(name=f"s_{e}") for e in ENGS}
        self.cnt = {e: 0 for e in ENGS}
        self.seen = {e: {} for e in ENGS}
        self.res = {}
        self.dsem = [nc.alloc_semaphore(name=f"s_dma{i}") for i in range(n_dma_sems + 28)]
        self.dval = [0] * (n_dma_sems + 28)
        self.drange = {'hw': (0, n_dma_sems), 'sw': (n_dma_sems, n_dma_sems + 28)}
        self.dnext = {'hw': 0, 'sw': n_dma_sems}
        self.nins = 0

    def _deps(self, r, w):
        waits = {}

        def add(ev):
            if ev is None:
                return
            s, v = ev
            k = id(s)
            if k not in waits or waits[k][1] < v:
                waits[k] = (s, v)
        for x in r:
            st = self.res.get(x)
            if st:
                add(st['w'])
        for x in w:
            st = self.res.get(x)
            if st:
                add(st['w'])
                for ev in st['r'].values():
                    add(ev)
        return waits

    def _record(self, ev, r, w):
        for x in w:
            self.res[x] = {'w': ev, 'r': {}}
        for x in r:
            st = self.res.setdefault(x, {'w': None, 'r': {}})
            k = id(ev[0])
            if k not in st['r'] or st['r'][k][1] < ev[1]:
                st['r'][k] = ev

    def op(self, eng, fn, r=(), w=(), dma=False):
        waits = self._deps(r, w)
        if dma:
            kind = 'sw' if eng == 'pool' else 'hw'
            lo, hi = self.drange[kind]
            i = self.dnext[kind]
            self.dnext[kind] = lo + (i + 1 - lo) % (hi - lo)
            s = self.dsem[i]
            if self.dval[i] > 0:
                waits[id(s)] = (s, self.dval[i])
            self.dval[i] += 16
            ev = (s, self.dval[i])
            incn = 16
        else:
            self.cnt[eng] += 1
            ev = (self.sem[eng], self.cnt[eng])
            incn = 1
        wl = []
        seen = self.seen[eng]
        for k, (s, v) in waits.items():
            if seen.get(k, 0) >= v:
                continue
            if eng == 'pe' and s is self.sem['pe']:
                continue
            seen[k] = v
            wl.append((s, v))
        self.ops[eng].append((wl, fn, ev[0], incn))
        self._record(ev, r, w)
        self.nins += 1
        return ev

    def raw(self, eng, fn):
        self.ops[eng].append(([], fn, None, 0))

    def barrier(self):
        evs = [(self.sem[e], self.cnt[e]) for e in ENGS if self.cnt[e] > 0]
        evs += [(s, v) for s, v in zip(self.dsem, self.dval) if v > 0]
        for e in ENGS:
            seen = self.seen[e]
            wl = []
            for (s, v) in evs:
                if seen.get(id(s), 0) >= v:
                    continue
                seen[id(s)] = v
                wl.append((s, v))
            if wl:
                self.ops[e].append((wl, None, None, 0))
        self.res = {}

    def finish(self):
        self.barrier()
        nc = self.nc
        with nc.Block() as block:
            def replay(e, lst):
                for (wl, fn, s, incn) in lst:
                    for (ws, wv) in wl:
                        e.wait_ge(ws, wv)
                    if fn is None:
                        continue
                    ins = fn(e)
                    if s is None:
                        continue
                    if isinstance(ins, (list, tuple)):
                        ins = ins[-1]
                    ins.then_inc(s, incn)

            @block.tensor
            def _(e):
                replay(e, self.ops['pe'])

            @block.scalar
            def _(e):
                replay(e, self.ops['act'])

            @block.vector
            def _(e):
                replay(e, self.ops['dve'])

            @block.gpsimd
            def _(e):
                replay(e, self.ops['pool'])

            @block.sync
            def _(e):
                replay(e, self.ops['sp'])


def sub_ap(ap, free):
    lst = [list(ap.ap[0])] + [[int(s), int(n)] for s, n in free]
    return bass.AP(ap.tensor, ap.offset, lst)


def rev_ap(ap2d):
    lst = [list(a) for a in ap2d.ap]
    st, n = lst[-1]
    lst[-1] = [-st, n]
    return bass.AP(ap2d.tensor, ap2d.offset + (n - 1) * st, lst)


class Builder:
    def __init__(self, plan, dbg=()):
        self.plan = plan
        self.nc = nc = bass.Bass("TRN2", target_bir_lowering=False)
        self.P = Prog(nc)
        self.dbg = set(dbg)
        self.din = {}
        self.uid = 0

    def inp(self, name, shape, dt=F32):
        h = self.nc.dram_tensor(name, list(shape), dt, kind="ExternalInput")
        self.din[name] = h
        return h.ap()

    def scratch(self, name, shape, dt=F32):
        kind = "ExternalOutput" if name in self.dbg else "Internal"
        return self.nc.dram_tensor(name, list(shape), dt, kind=kind).ap()

    def scratch_out(self, name, shape, dt=F32):
        return self.nc.dram_tensor(name, list(shape), dt, kind="ExternalOutput").ap()

    def free_dma_tmps(self, ins):
        g = self.nc.gpsimd
        RH = type(self.reg_off)
        for nm_ in set(re.findall(r"Pool_tmp_(\d+)", ins.concise())):
            n = int(nm_)
            g.free_register(RH(name=f"Pool_tmp_{n}", engine=self.reg_off.engine))
            for d_ in (1, 2, 3):
                try:
                    g.free_register(RH(name=f"Pool_Pool_moe_off_snap_{n - d_}", engine=self.reg_off.engine))
                    break
                except Exception:
                    pass

    def nm(self, p):
        self.uid += 1
        return f"{p}_{self.uid}"

    def dma(self, eng, out, in_, r, w):
        return self.P.op(eng, lambda e: e.dma_start(out=out, in_=in_), r=r, w=w, dma=True)

    SHAPES = {
        "x_in": [T, D], "cvec": [32, 128], "ident": [128, 128],
        "ada_w": [DEPTH, 2, D, 3 * D], "ada_b": [DEPTH, 2, 3 * D],
        "ln_g": [DEPTH, 2, D], "ln_b": [DEPTH, 2, D],
        "lru_w_in": [2, D, 2 * D], "lru_conv_w": [2, 4, D], "lru_conv_b": [2, D],
        "lru_gate_w": [2, 2, 2, 8, 256, 256], "lru_gate_b": [2, 2, 2, D], "lru_lambda": [2, 2, D],
        "lru_w_out": [2, D, D], "attn_w_qkv": [2, D, 2560], "attn_sinks": [2, 32], "attn_w_o": [2, D, D],
        "router_w": [DEPTH, D, NE], "router_b": [DEPTH, NE],
        "moe_w_gu": [DEPTH, NE, D, 2 * DE], "moe_b_gu": [DEPTH, NE, 2 * DE],
        "moe_w_down": [DEPTH, NE, DE, D], "moe_b_down": [DEPTH, NE, D],
        "rope_t": [S, 64], "tri": [3, 128, 128], "iota": [128, 512], "pcol": [128, 1],
    }

    def g(self, name):
        if name not in self.din:
            self.din[name] = self.nc.dram_tensor(name, list(self.SHAPES[name]), F32, kind="ExternalInput")
        return self.din[name].ap()

    def declare_io(self):
        nc = self.nc
        self.y_out = nc.dram_tensor("y_out", [S, D], F32, kind="ExternalOutput").ap()
        self.xs = self.scratch("xs", [T, D])
        self.modb = self.scratch("modb", [DEPTH, 2, 2, 128, 3 * D])
        self.uT = self.scratch("uT", [2 * D, T])
        self.mT = self.scratch("mT", [D, T], BF16)
        self.qTd = self.scratch("qTd", [NT, 128, 16, 128], BF16)
        self.xdisp = self.scratch("xdisp", [(T * 4 // 128 + NE) * 128, D], BF16)
        self.yslot = self.scratch("yslot", [(T * 4 // 128 + NE) * 128, D])

    def consts(self):
        nc, P = self.nc, self.P
        self.ident32 = nc.alloc_sbuf_tensor("ident32", [128, 128], F32)
        self.ident16 = nc.alloc_sbuf_tensor("ident16", [128, 128], BF16)
        self.ones32 = nc.alloc_sbuf_tensor("ones32", [128, 128], F32)
        self.ones16 = nc.alloc_sbuf_tensor("ones16", [128, 128], BF16)
        self.dma('sp', self.ident32[:], self.g('ident'), r=[], w=['ident32'])
        P.op('dve', lambda e: e.tensor_copy(out=self.ident16[:], in_=self.ident32[:]), r=['ident32'], w=['ident16'])
        P.op('pool', lambda e: e.memset(self.ones32[:], 1.0), w=['ones32'])
        P.op('pool', lambda e: e.memset(self.ones16[:], 1.0), w=['ones16'])
        self.reg_e = nc.gpsimd.alloc_register("moe_e")
        self.reg_off = nc.gpsimd.alloc_register("moe_off")
        self.eps_t = nc.alloc_sbuf_tensor("eps_t", [128, 1], F32)
        P.op('pool', lambda e: e.memset(self.eps_t[:], LN_EPS), w=['eps'])

    def bcast_rows(self, st, ps, rows, dst, tag):
        P = self.P
        N = dst.shape[-1]
        ps_ap, ps_res = ps
        for n0 in range(0, N, 512):
            n1 = min(N, n0 + 512)
            P.op('pe', lambda e, n0=n0, n1=n1: e.matmul(ps_ap[:, 0:n1 - n0], lhsT=self.ones32[0:1, :], rhs=rows[0:1, n0:n1], start=True, stop=True),
                 r=['ones32', tag + '_row'], w=[ps_res])
            P.op('act', lambda e, n0=n0, n1=n1: e.activation(out=dst[:, n0:n1], in_=ps_ap[:, 0:n1 - n0], func=AF.Copy),
                 r=[ps_res], w=[tag])

    def stage_ada(self, layers):
        nc, P = self.nc, self.P
        with ExitStack() as es:
            A = lambda *a: es.enter_context(nc.sbuf_tensor(self.nm(a[0]), *a[1:]))
            crow = A("ada_crow", [32, 128], F32)
            cT = A("ada_cT", [128, 32], F32)
            crep = A("ada_crep", [128, 2, 16, 128], BF16)
            wp = [A(f"ada_wp{i}", [128, 16, 512], BF16) for i in range(3)]
            brow = A("ada_brow", [1, 3 * D], BF16)
            stg = [A(f"ada_stg{i}", [128, 512], F32) for i in range(2)]
            ps = [es.enter_context(nc.psum_tensor(f"ada_ps{i}", [128, 512], F32)) for i in range(3)]
            self.dma('sp', crow[:], self.g('cvec'), r=[], w=['crow'])
            P.op('pe', lambda e: e.transpose(out=ps[2][:, 0:32], in_=crow[:], identity=self.ident32[0:32, 0:32]), r=['crow', 'ident32'], w=['aps2'])
            P.op('act', lambda e: e.activation(out=cT[:], in_=ps[2][:, 0:32], func=AF.Silu), r=['aps2'], w=['cT'])
            for v in range(2):
                src = sub_ap(cT[:, v * 16:(v + 1) * 16], [(1, 16), (0, 128)])
                P.op('dve', lambda e, v=v, src=src: e.tensor_copy(out=crep[:, v, :, :], in_=src), r=['cT'], w=[f'crep{v}'])
            k = 0
            kw = 0
            for i in layers:
                for s in range(2):
                    for h3 in range(3):
                        P.op('pool', lambda e, i=i, s=s, h3=h3: e.dma_start(out=brow[0:1, h3 * D:(h3 + 1) * D], in_=self.g('ada_b')[i, s:s + 1, h3 * D:(h3 + 1) * D]), r=[], w=[f'brow{h3}'], dma=True)
                    wsrc = self.g('ada_w')[i, s].rearrange("(c p) n -> p c n", p=128)
                    for pn in range(12):
                        wb = wp[kw % 3]
                        wr = f'wp{kw % 3}'
                        kw += 1
                        for h in range(4):
                            P.op('pool', lambda e, wb=wb, h=h, pn=pn, wsrc=wsrc: e.dma_start(out=wb[:, 4 * h:4 * h + 4, :], in_=wsrc[:, 4 * h:4 * h + 4, pn * 512:(pn + 1) * 512]), r=[], w=[wr + f'_{h}'], dma=True)
                        for v in range(2):
                            pt = ps[k % 2]
                            pr = f'aps{k % 2}'
                            for c in range(16):
                                P.op('pe', lambda e, pt=pt, wb=wb, v=v, c=c: e.matmul(pt[:], lhsT=crep[:, v, c, :], rhs=wb[:, c, :], start=(c == 0), stop=False),
                                     r=[f'crep{v}', wr + f'_{c // 4}'], w=[pr])
                            P.op('pe', lambda e, pt=pt, pn=pn: e.matmul(pt[:], lhsT=self.ones16[0:1, :], rhs=brow[0:1, pn * 512:(pn + 1) * 512], start=False, stop=True),
                                 r=['ones16'] + [f'brow{h3}' for h3 in range(3)], w=[pr])
                            sg = stg[k % 2]
                            sr = f'astg{k % 2}'
                            bias = 1.0 if 4 <= pn < 8 else 0.0
                            P.op('act', lambda e, sg=sg, pt=pt, bias=bias: e.activation(out=sg[:], in_=pt[:], func=AF.Identity, bias=bias, scale=1.0), r=[pr], w=[sr])
                            self.dma('sp', self.modb[i, s, v, :, pn * 512:(pn + 1) * 512], sg[:], r=[sr], w=[f'modb{i}{s}{v}'])
                            k += 1
            P.barrier()


    def ln_alloc(self, es, pfx):
        nc = self.nc
        A = lambda *a: es.enter_context(nc.sbuf_tensor(self.nm(a[0]), *a[1:]))
        L = {}
        L['lng'] = A(pfx + "_lng", [128, D], F32)
        L['lnb'] = A(pfx + "_lnb", [128, D], F32)
        L['row'] = A(pfx + "_row", [1, D], F32)
        L['st'] = [A(pfx + f"_st{i}", [128, 4, 6], F32) for i in range(2)]
        L['mv'] = [A(pfx + f"_mv{i}", [128, 8], F32) for i in range(2)]
        L['xn'] = [A(pfx + f"_xn{i}", [128, D], F32) for i in range(2)]
        L['k'] = 0
        return L

    def ln_load(self, L, i, s, ps):
        for nm, src in (('lng', self.g('ln_g')), ('lnb', self.g('ln_b'))):
            self.dma('sp', L['row'][:], src[i, s:s + 1, :], r=[], w=['ln_row'])
            self.bcast_rows(None, ps, L['row'], L[nm], 'ln')

    def ln_tile(self, L, pre, pre_res, dst, dst_res):
        P = self.P
        k = L['k'] % 2
        L['k'] += 1
        st, mv, xn = L['st'][k], L['mv'][k], L['xn'][k]
        sr, mr, xr = f'ln_st{k}', f'ln_mv{k}', f'ln_xn{k}'
        P.op('dve', lambda e: [e.bn_stats(out=st[:, j, :], in_=pre[:, j * 512:(j + 1) * 512]) for j in range(4)], r=[pre_res], w=[sr])
        P.op('dve', lambda e: e.bn_aggr(out=mv[:, 0:2], in_=st[:].rearrange("p a b -> p (a b)")), r=[sr], w=[mr])
        P.op('act', lambda e: e.activation(out=mv[:, 2:3], in_=mv[:, 1:2], func=AF.Sqrt, bias=self.eps_t[:, 0:1], scale=1.0), r=[mr, 'eps'], w=[mr + 'a'])
        P.op('dve', lambda e: e.reciprocal(out=mv[:, 3:4], in_=mv[:, 2:3]), r=[mr + 'a'], w=[mr + 'b'])
        P.op('dve', lambda e: e.tensor_scalar(out=mv[:, 4:5], in0=mv[:, 0:1], scalar1=mv[:, 3:4], scalar2=-1.0, op0=ALU.mult, op1=ALU.mult), r=[mr, mr + 'b'], w=[mr + 'c'])
        P.op('act', lambda e: e.activation(out=xn[:], in_=pre[:], func=AF.Identity, bias=mv[:, 4:5], scale=mv[:, 3:4]), r=[pre_res, mr + 'b', mr + 'c'], w=[xr])
        P.op('dve', lambda e: e.tensor_tensor(out=xn[:], in0=xn[:], in1=L['lng'][:], op=ALU.mult), r=['ln'], w=[xr])
        P.op('dve', lambda e: e.tensor_tensor(out=xn[:], in0=xn[:], in1=L['lnb'][:], op=ALU.add), r=['ln'], w=[xr])
        self.dma('sp', dst, xn[:], r=[xr], w=[dst_res])

    def stage_moe(self, i, srcname, dstname, need_ctx):
        nc, P = self.nc, self.P
        src = self.g('x_in') if srcname == 'x_in' else self.xs
        nt = NT if need_ctx else S // 128
        NBLK = (nt * 128 * 4) // 128 + NE
        BIG = float(DEPTH * NE)
        wgu_t = self.g('moe_w_gu').tensor
        wdn_t = self.g('moe_w_down').tensor
        bgu_t = self.g('moe_b_gu').tensor
        with ExitStack() as es0:
            A0 = lambda *a: es0.enter_context(nc.sbuf_tensor(self.nm(a[0]), *a[1:]))
            Sk = A0("moe_Sk", [128, NT * 4], I32)
            Gk = A0("moe_Gk", [128, NT * 4], F32)
            GD = A0("moe_GD", [128, NT, NE], F32)
            EIDX = A0("moe_eidx", [128, NBLK], I32)
            with ExitStack() as es:
                A = lambda *a: es.enter_context(nc.sbuf_tensor(self.nm(a[0]), *a[1:]))
                PS = lambda n, sh, dt: es.enter_context(nc.psum_tensor(self.nm(n), sh, dt))
                sc1 = [A(f"m1_sc{v}", [128, D], F32) for v in range(2)]
                shb = [A(f"m1_sh{v}", [128, D], F32) for v in range(2)]
                wr = A("m1_wr", [128, 16, NE], F32)
                brow = A("m1_brow", [1, NE], F32)
                brb = A("m1_brb", [128, NE], F32)
                iob = A("m1_iob", [128, NBLK], F32)
                pcol = A("m1_pcol", [128, 1], F32)
                tri32 = A("m1_tri32", [128, 128], F32)
                U16 = A("m1_U16", [128, 128], BF16)
                cm = A("m1_cm", [128, NE], F32)
                cm16 = A("m1_cm16", [128, NE], BF16)
                zer = A("m1_zer", [128, NE], F32)
                xt = [A(f"m1_xt{k}", [128, D], F32) for k in range(2)]
                a32 = [A(f"m1_a32{k}", [128, D], F32) for k in range(2)]
                a16 = A("m1_a16", [128, NT, D], BF16)
                aT = [A(f"m1_aT{k}", [128, 16, 128], F32) for k in range(2)]
                MK = A("m1_MK", [128, NT, NE], F32)
                PR = A("m1_PR", [128, NT, NE], F32)
                sm = [A(f"m1_sm{k}", [128, 8, NE], F32) for k in range(2)]
                m16 = [A(f"m1_m16{k}", [128, NE], BF16) for k in range(2)]
                sk = [A(f"m1_sk{k}", [128, 16], F32) for k in range(2)]
                fidx = A("m1_fidx", [128, NBLK, 16], F32)
                coff = A("m1_coff", [128, 16], F32)
                cw = A("m1_cw", [128, 6, NE], F32)
                cwi = A("m1_cwi", [128, NE], I32)
                eb = A("m1_eb", [128, 6, NBLK], F32)
                pT = [PS(f"m1_pT{k}", [128, 512], F32) for k in range(2)]
                pr = PS("m1_pr", [128, 512], F32)
                pp = PS("m1_pp", [128, 512], F32)
                for v in range(2):
                    self.dma('sp', sc1[v][:], self.modb[i, 1, v, :, D:2 * D], r=[], w=[f'sc1{v}'])
                    self.dma('act', shb[v][:], self.modb[i, 1, v, :, 0:D], r=[], w=[f'shb{v}'])
                self.dma('sp', wr[:], self.g('router_w')[i].rearrange("(c p) e -> p c e", p=128), r=[], w=['wr'])
                self.dma('sp', brow[:], self.g('router_b')[i:i + 1, :], r=[], w=['m1_row'])
                self.bcast_rows(None, (pr, 'pr'), brow, brb, 'm1')
                self.dma('sp', iob[:], self.g('iota')[:, 0:NBLK], r=[], w=['iob'])
                P.op('dve', lambda e: e.tensor_scalar(out=iob[:], in0=iob[:], scalar1=128.0, scalar2=None, op0=ALU.mult), r=[], w=['iob'])
                self.dma('sp', coff[:], self.g('iota')[:, 0:16], r=[], w=['coff'])
                P.op('dve', lambda e: e.tensor_scalar(out=coff[:], in0=coff[:], scalar1=128.0, scalar2=None, op0=ALU.mult), r=[], w=['coff'])
                self.dma('sp', pcol[:], self.g('pcol'), r=[], w=['pcol'])
                self.dma('sp', tri32[:], self.g('tri')[0], r=[], w=['tri32'])
                P.op('dve', lambda e: e.tensor_copy(out=U16[:], in_=tri32[:]), r=['tri32'], w=['U16'])
                P.op('pool', lambda e: e.memset(cm[:], 0.0), w=['cm'])
                P.op('pool', lambda e: e.memset(cm16[:], 0.0), w=['cm16'])
                P.op('pool', lambda e: e.memset(zer[:], 0.0), w=['zer'])
                for tt in range(nt):
                    v = 0 if tt < 16 else 1
                    k = tt % 2
                    X, A32, AT, SM, M16 = xt[k], a32[k], aT[k], sm[k], m16[k]
                    rx, ra32, raT, rsm = f'xt{k}', f'a32{k}', f'aT{k}', f'sm{k}'
                    self.dma('sp', X[:], src[tt * 128:(tt + 1) * 128, :], r=[f'xs{tt}'], w=[rx])
                    P.op('dve', lambda e, X=X, A32=A32, v=v: e.tensor_tensor(out=A32[:], in0=X[:], in1=sc1[v][:], op=ALU.mult), r=[rx, f'sc1{v}'], w=[ra32])
                    P.op('dve', lambda e, A32=A32, v=v: e.tensor_tensor(out=A32[:], in0=A32[:], in1=shb[v][:], op=ALU.add), r=[f'shb{v}'], w=[ra32])
                    P.op('act', lambda e, A32=A32, tt=tt: e.activation(out=a16[:, tt, :], in_=A32[:], func=AF.Copy), r=[ra32], w=[f'a16_{tt}'])
                    for q in range(4):
                        pt = pT[q % 2]
                        ptr = f'pT{q % 2}'
                        P.op('pe', lambda e, pt=pt, A32=A32, q=q: [e.transpose(out=pt[:, j * 128:(j + 1) * 128], in_=A32[:, (4 * q + j) * 128:(4 * q + j + 1) * 128], identity=self.ident32[:]) for j in range(4)],
                             r=[ra32, 'ident32'], w=[ptr])
                        dst = AT[:, 4 * q:4 * q + 4, :].rearrange("p a b -> p (a b)")
                        if q % 2 == 0:
                            P.op('act', lambda e, pt=pt, dst=dst: e.activation(out=dst, in_=pt[:], func=AF.Copy), r=[ptr], w=[raT + f'_{q}'])
                        else:
                            P.op('dve', lambda e, pt=pt, dst=dst: e.tensor_copy(out=dst, in_=pt[:]), r=[ptr], w=[raT + f'_{q}'])
                    P.op('pe', lambda e, AT=AT: [e.matmul(pr[:, 0:NE], lhsT=AT[:, c, :], rhs=wr[:, c, :], start=(c == 0), stop=(c == 15)) for c in range(16)],
                         r=[raT + f'_{q}' for q in range(4)] + ['wr'], w=['pr'])
                    lg, ex, exm = [SM[:, j, :] for j in range(3)]
                    mask = MK[:, tt, :]
                    gate = GD[:, tt, :]
                    mx8 = SM[:, 3, 0:8]
                    negm = SM[:, 3, 8:9]
                    ssum = SM[:, 3, 9:10]
                    rsum = SM[:, 3, 10:11]
                    rt = f'tok{tt}'
                    P.op('dve', lambda e, lg=lg: e.tensor_tensor(out=lg, in0=pr[:, 0:NE], in1=brb[:], op=ALU.add), r=['pr', 'm1'], w=[rsm])
                    P.op('dve', lambda e, lg=lg, mx8=mx8: e.max(out=mx8, in_=lg), r=[rsm], w=[rsm])
                    P.op('dve', lambda e, lg=lg, mx8=mx8, mask=mask: e.tensor_scalar(out=mask, in0=lg, scalar1=mx8[:, 3:4], scalar2=None, op0=ALU.is_ge), r=[rsm], w=[rt])
                    P.op('dve', lambda e, mx8=mx8, negm=negm: e.tensor_scalar(out=negm, in0=mx8[:, 0:1], scalar1=-1.0, scalar2=None, op0=ALU.mult), r=[rsm], w=[rsm])
                    P.op('act', lambda e, lg=lg, ex=ex, negm=negm: e.activation(out=ex, in_=lg, func=AF.Exp, bias=negm, scale=1.0), r=[rsm], w=[rsm])
                    P.op('dve', lambda e, ex=ex, mask=mask, exm=exm, ssum=ssum: e.scalar_tensor_tensor(out=exm, in0=ex, scalar=1.0, in1=mask, op0=ALU.mult, op1=ALU.mult, accum_out=ssum), r=[rsm, rt], w=[rsm])
                    P.op('dve', lambda e, ssum=ssum, rsum=rsum: e.reciprocal(out=rsum, in_=ssum), r=[rsm], w=[rsm])
                    P.op('dve', lambda e, exm=exm, gate=gate, rsum=rsum: e.tensor_scalar(out=gate, in0=exm, scalar1=rsum, scalar2=None, op0=ALU.mult), r=[rsm], w=[rt])
                    P.op('dve', lambda e, mask=mask, M16=M16: e.tensor_copy(out=M16[:], in_=mask), r=[rt], w=[f'm16{k}'])
                    P.op('pe', lambda e, M16=M16: [e.matmul(pp[:, 0:NE], lhsT=U16[:], rhs=M16[:], start=True, stop=False),
                                                   e.matmul(pp[:, 0:NE], lhsT=self.ones16[:], rhs=cm16[:], start=False, stop=True)],
                         r=['U16', f'm16{k}', 'ones16', 'cm16'], w=['pp'])
                    P.op('dve', lambda e, tt=tt: e.tensor_copy(out=PR[:, tt, :], in_=pp[:, 0:NE]), r=['pp'], w=[rt])
                    P.op('dve', lambda e, mask=mask: e.tensor_tensor(out=cm[:], in0=cm[:], in1=mask, op=ALU.add), r=[rt], w=['cm'])
                    P.op('dve', lambda e: e.tensor_copy(out=cm16[:], in_=cm[:]), r=['cm'], w=['cm16'])
                cnt, pad, ends, pst = [cw[:, j, :] for j in range(4)]
                P.op('pe', lambda e: e.matmul(pp[:, 0:NE], lhsT=self.ones16[:], rhs=cm16[:], start=True, stop=True), r=['ones16', 'cm16'], w=['pp'])
                P.op('dve', lambda e: e.tensor_scalar(out=cnt, in0=pp[:, 0:NE], scalar1=127.0, scalar2=None, op0=ALU.add), r=['pp'], w=['cw'])
                P.op('dve', lambda e: e.tensor_copy(out=cwi[:], in_=cnt), r=['cw'], w=['cwi'])
                P.op('dve', lambda e: e.tensor_single_scalar(out=cwi[:], in_=cwi[:], scalar=7, op=ALU.arith_shift_right), r=[], w=['cwi'])
                P.op('dve', lambda e: e.tensor_single_scalar(out=cwi[:], in_=cwi[:], scalar=7, op=ALU.logical_shift_left), r=[], w=['cwi'])
                P.op('dve', lambda e: e.tensor_copy(out=pad, in_=cwi[:]), r=['cwi'], w=['cw'])
                P.op('dve', lambda e: e.tensor_tensor_scan(out=ends, data0=pad, data1=zer[:], initial=0.0, op0=ALU.add, op1=ALU.add), r=['zer'], w=['cw'])
                P.op('dve', lambda e: e.tensor_tensor(out=pst, in0=ends, in1=pad, op=ALU.subtract), r=[], w=['cw'])
                Eb, Em2, need, bas, tmp = [eb[:, j, :] for j in range(5)]
                P.op('pool', lambda e: e.memset(eb[:], 0.0), w=['eb'])
                for ee in range(NE):
                    P.op('dve', lambda e, ee=ee: e.scalar_tensor_tensor(out=Eb, in0=iob[:], scalar=cw[:, 2, ee:ee + 1], in1=Eb, op0=ALU.is_ge, op1=ALU.add), r=['iob', 'cw'], w=['eb'])
                P.op('dve', lambda e: e.tensor_scalar(out=Eb, in0=Eb, scalar1=float(NE - 1), scalar2=None, op0=ALU.min), r=[], w=['eb'])
                P.op('dve', lambda e: e.memset(need, 1.0), w=['eb'])
                P.op('dve', lambda e: e.tensor_tensor(out=eb[:, 2, 1:NBLK], in0=eb[:, 0, 1:NBLK], in1=eb[:, 0, 0:NBLK - 1], op=ALU.not_equal), r=[], w=['eb'])
                P.op('dve', lambda e: e.memset(eb[:, 2, NBLK // 2:NBLK // 2 + 1], 1.0), w=['eb'])

                def mk_index(dst_i32, nchunk, rows_per_e, with_p):
                    P.op('dve', lambda e: e.tensor_scalar(out=bas, in0=Eb, scalar1=float(rows_per_e), scalar2=float(i * NE * rows_per_e) - BIG, op0=ALU.mult, op1=ALU.add), r=[], w=['eb'])
                    if with_p:
                        P.op('dve', lambda e: e.tensor_scalar(out=bas, in0=bas, scalar1=pcol[:, 0:1], scalar2=None, op0=ALU.add), r=['pcol'], w=['eb'])
                    P.op('dve', lambda e: e.tensor_tensor(out=bas, in0=bas, in1=need, op=ALU.mult), r=[], w=['eb'])
                    P.op('dve', lambda e: e.tensor_scalar(out=bas, in0=bas, scalar1=BIG, scalar2=None, op0=ALU.add), r=[], w=['eb'])
                    if nchunk == 1:
                        P.op('dve', lambda e: e.tensor_copy(out=dst_i32[:], in_=bas), r=['eb'], w=['idx'])
                    else:
                        fv = fidx[:, :, 0:nchunk]
                        P.op('dve', lambda e: e.tensor_tensor(out=fv, in0=sub_ap(bas, [(1, NBLK), (0, nchunk)]), in1=sub_ap(coff[:, 0:nchunk], [(0, NBLK), (1, nchunk)]), op=ALU.add), r=['coff'], w=['fidx'])
                        P.op('dve', lambda e: e.tensor_copy(out=dst_i32[:], in_=fv), r=['fidx'], w=['idx'])
                mk_index(EIDX, 1, 1, False)
                for tt in range(nt):
                    k = tt % 2
                    SM, SKF = sm[k], sk[k]
                    rsm = f'smb{k}'
                    rt = f'tok{tt}'
                    mask = MK[:, tt, :]
                    gate = GD[:, tt, :]
                    sful, cs, oh, junk = [SM[:, j, :] for j in range(4, 8)]
                    P.op('dve', lambda e, sful=sful, tt=tt: e.tensor_tensor(out=sful, in0=PR[:, tt, :], in1=pst, op=ALU.add), r=[rt, 'cw'], w=[rsm])
                    P.op('dve', lambda e, cs=cs, mask=mask: e.tensor_tensor_scan(out=cs, data0=mask, data1=zer[:], initial=0.0, op0=ALU.add, op1=ALU.add), r=[rt, 'zer'], w=[rsm])
                    for kk in range(4):
                        P.op('dve', lambda e, oh=oh, cs=cs, mask=mask, kk=kk: e.scalar_tensor_tensor(out=oh, in0=cs, scalar=float(kk + 1), in1=mask, op0=ALU.is_equal, op1=ALU.mult), r=[rsm], w=[rsm])
                        P.op('dve', lambda e, oh=oh, sful=sful, junk=junk, SKF=SKF, kk=kk: e.scalar_tensor_tensor(out=junk, in0=oh, scalar=1.0, in1=sful, op0=ALU.mult, op1=ALU.mult, accum_out=SKF[:, kk:kk + 1]), r=[rsm], w=[rsm, f'skf{k}'])
                        P.op('dve', lambda e, oh=oh, gate=gate, junk=junk, kk=kk, tt=tt: e.scalar_tensor_tensor(out=junk, in0=oh, scalar=1.0, in1=gate, op0=ALU.mult, op1=ALU.mult, accum_out=Gk[:, tt * 4 + kk:tt * 4 + kk + 1]), r=[rsm], w=[rsm, 'Gk'])
                    P.op('dve', lambda e, SKF=SKF, tt=tt: e.tensor_copy(out=Sk[:, tt * 4:tt * 4 + 4], in_=SKF[:, 0:4]), r=[f'skf{k}'], w=[f'Sk{tt}'])
                    for kk in range(4):
                        P.op('pool', lambda e, tt=tt, kk=kk: e.indirect_dma_start(out=self.xdisp, out_offset=bass.IndirectOffsetOnAxis(ap=Sk[:, tt * 4 + kk:tt * 4 + kk + 1], axis=0), in_=a16[:, tt, :], in_offset=None),
                             r=[f'a16_{tt}', f'Sk{tt}'], w=[], dma=True)
                if 'dbg_moe' in self.dbg:
                    self.dbg_sk = self.scratch_out("dbg_sk", [128, NT * 4], I32)
                    self.dbg_eb = self.scratch_out("dbg_eb", [128, 6, NBLK], F32)
                    self.dbg_cw = self.scratch_out("dbg_cw", [128, 6, NE], F32)
                    self.dbg_ig = self.scratch_out("dbg_ig", [128, NBLK], I32)
                    self.dma('sp', self.dbg_sk, Sk[:], r=[f'Sk{t_}' for t_ in range(nt)], w=[])
                    self.dma('sp', self.dbg_eb, eb[:], r=['eb'], w=[])
                    self.dma('sp', self.dbg_cw, cw[:], r=['cw'], w=[])
                    self.dma('sp', self.dbg_ig, EIDX[:], r=['idx'], w=[])
                P.barrier()
            with ExitStack() as es:
                A = lambda *a: es.enter_context(nc.sbuf_tensor(self.nm(a[0]), *a[1:]))
                PS = lambda n, sh, dt: es.enter_context(nc.psum_tensor(self.nm(n), sh, dt))
                WGU = [A(f"m2_wgu{k}", [128, 16, 2 * DE], BF16) for k in range(2)]
                WDN = [A(f"m2_wdn{k}", [128, 6, D], BF16) for k in range(2)]
                BG = [A(f"m2_bg{k}", [128, 2 * DE], BF16) for k in range(2)]
                xin = [A(f"m2_xin{k}", [128, D], BF16) for k in range(2)]
                xT = [A(f"m2_xT{k}", [128, 16, 128], BF16) for k in range(2)]
                g32 = [A(f"m2_g32{k}", [128, DE], F32) for k in range(2)]
                sg = [A(f"m2_sg{k}", [128, DE], F32) for k in range(2)]
                u1 = [A(f"m2_u1{k}", [128, DE], F32) for k in range(2)]
                y16 = [A(f"m2_y16{k}", [128, DE], BF16) for k in range(2)]
                yT = [A(f"m2_yT{k}", [128, 6, 128], BF16) for k in range(2)]
                ostg = [A(f"m2_ostg{k}", [128, 1024], F32) for k in range(2)]
                pTb = [PS(f"m2_pTb{k}", [128, 1024], BF16) for k in range(2)]
                ph = [PS(f"m2_ph{k}", [128, 512], F32) for k in range(4)]
                pd = [PS(f"m2_pd{k}", [128, 512], F32) for k in range(2)]
                HALF = NBLK // 2

                def front(b):
                    par = b % 2
                    k = b % 2
                    sb = (b % 2) * HALF + b // 2
                    Wg, Bg, XIN, XT = WGU[par], BG[par], xin[k], xT[k]
                    Wd = WDN[par]
                    G32, SG, U1, Y16 = g32[k], sg[k], u1[k], y16[k]
                    rwg, rwd, rbg = f'wgu{par}', f'wdn{par}', f'bg{par}'

                    def ld(e, b=b, out=None, tensor=None, per_e=0, pat=None):
                        e.reg_load(self.reg_e, EIDX[0:1, sb:sb + 1])
                        e.reg_mul(self.reg_off, self.reg_e, per_e)
                        ins = e.dma_start(out=out, in_=bass.AP(tensor, self.reg_off, pat), bounds_check="skip_entire_dma")
                        self.free_dma_tmps(ins)
                        return ins
                    for qq in range(4):
                        def ldq(e, qq=qq):
                            e.reg_load(self.reg_e, EIDX[0:1, sb:sb + 1])
                            e.reg_mul(self.reg_off, self.reg_e, D * 2 * DE)
                            e.reg_add(self.reg_off, self.reg_off, qq * 4 * 128 * 2 * DE)
                            ins = e.dma_start(out=Wg[:, 4 * qq:4 * qq + 4, :], in_=bass.AP(wgu_t, self.reg_off, [[2 * DE, 128], [128 * 2 * DE, 4], [1, 2 * DE]]), bounds_check="skip_entire_dma")
                            self.free_dma_tmps(ins)
                            return ins
                        P.op('pool', ldq, r=[], w=[rwg + f'_{qq}'], dma=True)
                    P.op('pool', lambda e: ld(e, b, Bg[0:1, :], bgu_t, 2 * DE, [[2 * DE, 1], [1, 2 * DE]]), r=[], w=[rbg], dma=True)
                    P.op('pool', lambda e: ld(e, b, Wd[:, :, :], wdn_t, DE * D, [[D, 128], [128 * D, 6], [1, D]]), r=[], w=[rwd], dma=True)
                    for hh in range(2):
                        pt = pTb[hh]
                        ptr = f'pTb{hh}'
                        P.op('pe', lambda e, pt=pt, hh=hh: [e.transpose(out=pt[:, j * 128:(j + 1) * 128], in_=XIN[:, (8 * hh + j) * 128:(8 * hh + j + 1) * 128], identity=self.ident16[:]) for j in range(8)],
                             r=[f'xin{k}', 'ident16'], w=[ptr])
                        dst = XT[:, 8 * hh:8 * hh + 8, :].rearrange("p a b -> p (a b)")
                        if hh == 0:
                            P.op('act', lambda e, dst=dst, pt=pt: e.activation(out=dst, in_=pt[:], func=AF.Copy), r=[ptr], w=[f'xT{k}_{hh}'])
                        else:
                            P.op('dve', lambda e, dst=dst, pt=pt: e.tensor_copy(out=dst, in_=pt[:]), r=[ptr], w=[f'xT{k}_{hh}'])
                    segs = [(ph[0], 0, 512, 0, 'ph0'), (ph[1], 0, 512, DE, 'ph1'), (ph[2], 0, 256, 512, 'ph2'), (ph[3], 0, 256, DE + 512, 'ph3')]

                    def gu_group(e, qq):
                        out = []
                        for c in range(4 * qq, 4 * qq + 4):
                            for (pt_, o0, n_, w0, _) in segs:
                                out.append(e.matmul(pt_[:, o0:o0 + n_], lhsT=XT[:, c, :], rhs=Wg[:, c, w0:w0 + n_], start=(c == 0), stop=False))
                        if qq == 3:
                            for (pt_, o0, n_, w0, _) in segs:
                                out.append(e.matmul(pt_[:, o0:o0 + n_], lhsT=self.ones16[0:1, :], rhs=Bg[0:1, w0:w0 + n_], start=False, stop=True))
                        return out
                    for qq in range(4):
                        P.op('pe', lambda e, qq=qq: gu_group(e, qq), r=[f'xT{k}_0', f'xT{k}_1', 'ones16', rwg + f'_{qq}'] + ([rbg] if qq == 3 else []), w=['ph0', 'ph1', 'ph2', 'ph3'])
                    P.op('dve', lambda e: e.tensor_scalar(out=G32[:, 0:512], in0=ph[0][:], scalar1=7.0, scalar2=None, op0=ALU.min), r=['ph0'], w=[f'g32a{k}'])
                    P.op('dve', lambda e: e.tensor_scalar(out=G32[:, 512:DE], in0=ph[2][:, 0:256], scalar1=7.0, scalar2=None, op0=ALU.min), r=['ph2'], w=[f'g32b{k}'])
                    P.op('dve', lambda e: e.tensor_scalar(out=U1[:, 0:512], in0=ph[1][:], scalar1=7.0, scalar2=-7.0, op0=ALU.min, op1=ALU.max), r=['ph1'], w=[f'u1a{k}'])
                    P.op('dve', lambda e: e.tensor_scalar(out=U1[:, 512:DE], in0=ph[3][:, 0:256], scalar1=7.0, scalar2=-7.0, op0=ALU.min, op1=ALU.max), r=['ph3'], w=[f'u1b{k}'])
                    P.op('act', lambda e: e.activation(out=SG[:], in_=G32[:], func=AF.Sigmoid, scale=1.702), r=[f'g32a{k}', f'g32b{k}'], w=[f'sg{k}'])
                    P.op('dve', lambda e: e.tensor_tensor(out=SG[:], in0=SG[:], in1=G32[:], op=ALU.mult), r=[f'g32a{k}', f'g32b{k}'], w=[f'sg{k}'])
                    P.op('dve', lambda e: e.scalar_tensor_tensor(out=Y16[:], in0=U1[:], scalar=1.0, in1=SG[:], op0=ALU.add, op1=ALU.mult), r=[f'u1a{k}', f'u1b{k}', f'sg{k}'], w=[f'y16{k}'])

                def back(b):
                    par = b % 2
                    k = b % 2
                    sb = (b % 2) * HALF + b // 2
                    Wd, YT, Y16 = WDN[par], yT[k], y16[k]
                    rwd = f'wdn{par}'
                    P.op('pe', lambda e: [e.transpose(out=pTb[0][:, j * 128:(j + 1) * 128], in_=Y16[:, j * 128:(j + 1) * 128], identity=self.ident16[:]) for j in range(6)],
                         r=[f'y16{k}', 'ident16'], w=['pTb0'])
                    P.op('act', lambda e: e.activation(out=YT[:, :, :].rearrange("p a b -> p (a b)"), in_=pTb[0][:, 0:DE], func=AF.Copy), r=['pTb0'], w=[f'yT{k}'])
                    for hf in range(2):
                        OS = ostg[hf]
                        for q in range(2):
                            np_ = hf * 2 + q
                            pdt = pd[q]
                            pdr = f'pd{q}'
                            P.op('pe', lambda e, pdt=pdt, np_=np_: [e.matmul(pdt[:], lhsT=YT[:, j, :], rhs=Wd[:, j, np_ * 512:(np_ + 1) * 512], start=(j == 0), stop=(j == 5)) for j in range(6)],
                                 r=[f'yT{k}', rwd], w=[pdr])
                            P.op('act', lambda e, OS=OS, pdt=pdt, q=q: e.activation(out=OS[:, q * 512:(q + 1) * 512], in_=pdt[:], func=AF.Copy), r=[pdr], w=[f'ostg{hf}'])
                        self.dma('sp', self.yslot[sb * 128:(sb + 1) * 128, hf * 1024:(hf + 1) * 1024], OS[:], r=[f'ostg{hf}'], w=[])

                def xload(b):
                    sb_ = (b % 2) * HALF + b // 2
                    self.dma('sp', xin[b % 2][:], self.xdisp[sb_ * 128:(sb_ + 1) * 128, :], r=[], w=[f'xin{b % 2}'])
                xload(0)
                for b in range(NBLK + 1):
                    if b + 1 < NBLK:
                        xload(b + 1)
                    if b < NBLK:
                        front(b)
                    if b >= 1:
                        back(b - 1)
                P.barrier()
            with ExitStack() as es:
                A = lambda *a: es.enter_context(nc.sbuf_tensor(self.nm(a[0]), *a[1:]))
                PS = lambda n, sh, dt: es.enter_context(nc.psum_tensor(self.nm(n), sh, dt))
                gb = [A(f"m3_gb{v}", [128, D], F32) for v in range(2)]
                L = self.ln_alloc(es, "m3")
                rows = [[A(f"m3_r{k}_{j}", [128, D], F32) for j in range(4)] for k in range(2)]
                xt = [A(f"m3_xt{k}", [128, D], F32) for k in range(2)]
                acc = [A(f"m3_acc{k}", [128, D], F32) for k in range(2)]
                bdn = A("m3_bdn", [NE, D], F32)
                gT = [A(f"m3_gT{k}", [NE, 128], F32) for k in range(2)]
                dg = [A(f"m3_dg{k}", [128, 4, 128], F32) for k in range(2)]
                ps = PS("m3_ps", [128, 512], F32)
                pgt = PS("m3_pgt", [128, 512], F32)
                pb = [PS(f"m3_pb{k}", [128, 512], F32) for k in range(4)]
                for v in range(2):
                    self.dma('sp', gb[v][:], self.modb[i, 1, v, :, 2 * D:3 * D], r=[], w=[f'gb{v}'])
                self.dma('sp', bdn[:], self.g('moe_b_down')[i], r=[], w=['bdn'])
                self.ln_load(L, i, 1, (ps, 'm3ps'))
                for tt in range(nt):
                    v = 0 if tt < 16 else 1
                    k = tt % 2
                    R, X, AC, GT = rows[k], xt[k], acc[k], gT[k]
                    for j in range(4):
                        P.op('pool', lambda e, R=R, j=j, tt=tt: e.indirect_dma_start(out=R[j][:, :], out_offset=None, in_=self.yslot, in_offset=bass.IndirectOffsetOnAxis(ap=Sk[:, tt * 4 + j:tt * 4 + j + 1], axis=0)),
                             r=[], w=[f'r{k}_{j}'], dma=True)
                    self.dma('act', X[:], src[tt * 128:(tt + 1) * 128, :], r=[f'xs{tt}'], w=[f'x3{k}'])
                    P.op('pe', lambda e, tt=tt: e.transpose(out=pgt[0:NE, 0:128], in_=GD[:, tt, :], identity=self.ident32[:]), r=['ident32'], w=['pgt'])
                    P.op('act', lambda e, GT=GT: e.activation(out=GT[:], in_=pgt[0:NE, 0:128], func=AF.Copy), r=['pgt'], w=[f'gT{k}'])
                    DG = dg[k]
                    for j in range(4):
                        P.op('act', lambda e, DG=DG, j=j, tt=tt: e.activation(out=DG[:, j, :], in_=self.ident32[:], func=AF.Identity, scale=Gk[:, tt * 4 + j:tt * 4 + j + 1]), r=['ident32'], w=[f'dg{k}_{j}'])
                    for q in range(4):
                        P.op('pe', lambda e, DG=DG, R=R, GT=GT, q=q: [e.matmul(pb[q][:], lhsT=DG[:, j, :], rhs=R[j][:, q * 512:(q + 1) * 512], start=(j == 0), stop=False) for j in range(4)]
                             + [e.matmul(pb[q][:], lhsT=GT[:, :], rhs=bdn[:, q * 512:(q + 1) * 512], start=False, stop=True)],
                             r=[f'gT{k}', 'bdn'] + [f'dg{k}_{j}' for j in range(4)] + [f'r{k}_{j}' for j in range(4)], w=[f'pb{q}'])
                        P.op('dve', lambda e, AC=AC, q=q, v=v: e.tensor_tensor(out=AC[:, q * 512:(q + 1) * 512], in0=pb[q][:], in1=gb[v][:, q * 512:(q + 1) * 512], op=ALU.mult), r=[f'pb{q}', f'gb{v}'], w=[f'acc{k}'])
                    P.op('dve', lambda e, AC=AC, X=X: e.scalar_tensor_tensor(out=AC[:], in0=X[:], scalar=float(DN_ALPHA), in1=AC[:], op0=ALU.mult, op1=ALU.add), r=[f'x3{k}'], w=[f'acc{k}'])
                    if dstname == 'y_out':
                        dst = self.y_out[tt * 128:(tt + 1) * 128, :]
                    else:
                        dst = self.xs[tt * 128:(tt + 1) * 128, :]
                    self.ln_tile(L, AC, f'acc{k}', dst, f'xs{tt}')
                P.barrier()


    def stage_proj(self, i, wname, j, srcname, need_ctx):
        nc, P = self.nc, self.P
        src = self.g('x_in') if srcname == 'x_in' else self.xs
        nt = NT if need_ctx else S // 128
        with ExitStack() as es:
            A = lambda *a: es.enter_context(nc.sbuf_tensor(self.nm(a[0]), *a[1:]))
            PS = lambda n, sh, dt: es.enter_context(nc.psum_tensor(self.nm(n), sh, dt))
            wo = A("pj_wo", [128, 16, D], BF16)
            gb = [A(f"pj_gb{v}", [128, D], F32) for v in range(2)]
            L = self.ln_alloc(es, "pj")
            mt = [A(f"pj_mt{k}", [128, 16, 512], BF16) for k in range(2)]
            xt = [A(f"pj_xt{k}", [128, D], F32) for k in range(2)]
            acc = [A(f"pj_acc{k}", [128, D], F32) for k in range(2)]
            po = [PS(f"pj_po{k}", [128, 512], F32) for k in range(4)]
            ps = PS("pj_ps", [128, 512], F32)
            wsrc = self.g(wname)[j].rearrange("(c p) n -> p c n", p=128)
            for h in range(4):
                P.op('pool', lambda e, h=h: e.dma_start(out=wo[:, 4 * h:4 * h + 4, :], in_=wsrc[:, 4 * h:4 * h + 4, :]), r=[], w=[f'wo{h}'], dma=True)
            for v in range(2):
                self.dma('sp', gb[v][:], self.modb[i, 0, v, :, 2 * D:3 * D], r=[], w=[f'gb{v}'])
            self.ln_load(L, i, 0, (ps, 'pjps'))
            msrc = self.mT.rearrange("(c p) t -> p c t", p=128)
            for tt in range(nt):
                v = 0 if tt < 16 else 1
                k = tt % 2
                g4 = tt // 4
                MT = mt[g4 % 2]
                if tt % 4 == 0:
                    n_tok = min(512, nt * 128 - g4 * 512)
                    for h in range(2):
                        self.dma('sp' if h == 0 else 'act', MT[:, 8 * h:8 * h + 8, 0:n_tok], msrc[:, 8 * h:8 * h + 8, g4 * 512:g4 * 512 + n_tok], r=[], w=[f'mt{g4 % 2}_{h}'])
                X, AC = xt[k], acc[k]
                self.dma('act', X[:], src[tt * 128:(tt + 1) * 128, :], r=[f'xs{tt}'], w=[f'pjx{k}'])
                t0 = (tt % 4) * 128
                for q in range(4):
                    P.op('pe', lambda e, MT=MT, q=q, t0=t0: [e.matmul(po[q][:], lhsT=MT[:, c, t0:t0 + 128], rhs=wo[:, c, q * 512:(q + 1) * 512], start=(c == 0), stop=(c == 15)) for c in range(16)],
                         r=[f'mt{g4 % 2}_0', f'mt{g4 % 2}_1'] + [f'wo{h}' for h in range(4)], w=[f'po{q}'])
                    P.op('dve', lambda e, AC=AC, q=q, v=v: e.tensor_tensor(out=AC[:, q * 512:(q + 1) * 512], in0=po[q][:], in1=gb[v][:, q * 512:(q + 1) * 512], op=ALU.mult), r=[f'po{q}', f'gb{v}'], w=[f'pjacc{k}'])
                P.op('dve', lambda e, AC=AC, X=X: e.scalar_tensor_tensor(out=AC[:], in0=X[:], scalar=float(DN_ALPHA), in1=AC[:], op0=ALU.mult, op1=ALU.add), r=[f'pjx{k}'], w=[f'pjacc{k}'])
                self.ln_tile(L, AC, f'pjacc{k}', self.xs[tt * 128:(tt + 1) * 128, :], f'xs{tt}')
            P.barrier()

    def build_aT(self, es, i, src, aT, nt, pfx):
        nc, P = self.nc, self.P
        A = lambda *a: es.enter_context(nc.sbuf_tensor(self.nm(a[0]), *a[1:]))
        PS = lambda n, sh, dt: es.enter_context(nc.psum_tensor(self.nm(n), sh, dt))
        sc1 = [A(f"{pfx}_sc{v}", [128, D], F32) for v in range(2)]
        shb = [A(f"{pfx}_sh{v}", [128, D], F32) for v in range(2)]
        xt = [A(f"{pfx}_xt{k}", [128, D], F32) for k in range(2)]
        a16 = [A(f"{pfx}_a16{k}", [128, D], BF16) for k in range(2)]
        pTb = [PS(f"{pfx}_pTb{k}", [128, 1024], BF16) for k in range(2)]
        for v in range(2):
            self.dma('sp', sc1[v][:], self.modb[i, 0, v, :, D:2 * D], r=[], w=[f'sc1{v}'])
            self.dma('act', shb[v][:], self.modb[i, 0, v, :, 0:D], r=[], w=[f'shb{v}'])
        for tt in range(nt):
            v = 0 if tt < 16 else 1
            k = tt % 2
            X, A16 = xt[k], a16[k]
            self.dma('sp', X[:], src[tt * 128:(tt + 1) * 128, :], r=[f'xs{tt}'], w=[f'atx{k}'])
            P.op('dve', lambda e, X=X, v=v: e.tensor_tensor(out=X[:], in0=X[:], in1=sc1[v][:], op=ALU.mult), r=[f'sc1{v}'], w=[f'atx{k}'])
            P.op('dve', lambda e, X=X, v=v: e.tensor_tensor(out=X[:], in0=X[:], in1=shb[v][:], op=ALU.add), r=[f'shb{v}'], w=[f'atx{k}'])
            P.op('act', lambda e, X=X, A16=A16: e.activation(out=A16[:], in_=X[:], func=AF.Copy), r=[f'atx{k}'], w=[f'ata{k}'])
            for hh in range(2):
                pt = pTb[hh]
                P.op('pe', lambda e, pt=pt, A16=A16, hh=hh: [e.transpose(out=pt[:, jj * 128:(jj + 1) * 128], in_=A16[:, (8 * hh + jj) * 128:(8 * hh + jj + 1) * 128], identity=self.ident16[:]) for jj in range(8)],
                     r=[f'ata{k}', 'ident16'], w=[f'atp{hh}'])
                dst = aT[:, 8 * hh:8 * hh + 8, tt * 128:(tt + 1) * 128]
                srcp = pt[:, :].rearrange("p (a b) -> p a b", a=8)
                if hh == 0:
                    P.op('act', lambda e, dst=dst, srcp=srcp: e.activation(out=dst, in_=srcp, func=AF.Copy), r=[f'atp{hh}'], w=[f'aT{tt}_{hh}'])
                else:
                    P.op('dve', lambda e, dst=dst, srcp=srcp: e.tensor_copy(out=dst, in_=srcp), r=[f'atp{hh}'], w=[f'aT{tt}_{hh}'])
        return [f'aT{tt}_{hh}' for tt in range(nt) for hh in range(2)]

    def stage_lru(self, j, i, srcname):
        nc, P = self.nc, self.P
        src = self.g('x_in') if srcname == 'x_in' else self.xs
        with ExitStack() as es:
            A = lambda *a: es.enter_context(nc.sbuf_tensor(self.nm(a[0]), *a[1:]))
            PS = lambda n, sh, dt: es.enter_context(nc.psum_tensor(self.nm(n), sh, dt))
            aT = A("l1_aT", [128, 16, T], BF16)
            wpan = [A(f"l1_wp{k}", [128, 16, 512], BF16) for k in range(2)]
            ustg = [A(f"l1_us{k}", [128, T], F32) for k in range(2)]
            aT_res = self.build_aT(es, i, src, aT, NT, "l1")
            pu = [PS(f"l1_pu{k}", [128, 512], F32) for k in range(5)]
            wsrc = self.g('lru_w_in')[j].rearrange("(c p) n -> p c n", p=128)
            pieces = [(0, 512), (512, 512), (1024, 512), (1536, 512), (2048, 256)]
            nev = 0
            for pn in range(8):
                WP = wpan[pn % 2]
                for h in range(2):
                    P.op('pool', lambda e, WP=WP, h=h, pn=pn: e.dma_start(out=WP[:, 8 * h:8 * h + 8, :], in_=wsrc[:, 8 * h:8 * h + 8, pn * 512:(pn + 1) * 512]), r=[], w=[f'wp{pn % 2}_{h}'], dma=True)
                for fc in range(4):
                    F = pn * 4 + fc
                    US = ustg[F % 2]
                    for pc, (p0, n_) in enumerate(pieces):
                        P.op('pe', lambda e, WP=WP, fc=fc, pc=pc, p0=p0, n_=n_: [e.matmul(pu[pc][:, 0:n_], lhsT=WP[:, c, fc * 128:(fc + 1) * 128], rhs=aT[:, c, p0:p0 + n_], start=(c == 0), stop=(c == 15)) for c in range(16)],
                             r=[f'wp{pn % 2}_0', f'wp{pn % 2}_1'] + (aT_res if (pn == 0 and fc == 0) else []), w=[f'pu{pc}'])
                        if nev % 2 == 0:
                            P.op('act', lambda e, US=US, pc=pc, p0=p0, n_=n_: e.activation(out=US[:, p0:p0 + n_], in_=pu[pc][:, 0:n_], func=AF.Copy), r=[f'pu{pc}'], w=[f'us{F % 2}'])
                        else:
                            P.op('dve', lambda e, US=US, pc=pc, p0=p0, n_=n_: e.tensor_copy(out=US[:, p0:p0 + n_], in_=pu[pc][:, 0:n_]), r=[f'pu{pc}'], w=[f'us{F % 2}'])
                        nev += 1
                    self.dma('sp', self.uT[F * 128:(F + 1) * 128, :], US[:], r=[f'us{F % 2}'], w=[])
            P.barrier()
        W = 2309
        LAT0, LAT1, CTX0, CTX1 = 1, 2049, 2051, 2307
        NW = CTX1 - LAT0
        with ExitStack() as es:
            A = lambda *a: es.enter_context(nc.sbuf_tensor(self.nm(a[0]), *a[1:]))
            PS = lambda n, sh, dt: es.enter_context(nc.psum_tensor(self.nm(n), sh, dt))
            prow = [A(f"l2_prow{k}", [128, 128], F32) for k in range(2)]
            pc_ = A("l2_pc", [128, 256], F32)
            cexp = A("l2_cexp", [128, 64], F32)
            rp = [A(f"l2_rp{k}", [128, 2, W], F32) for k in range(2)]
            ug = [A("l2_ug", [128, 2, T], F32)] * 2
            rc = A("l2_rc", [128, 2, W], F32)
            rc16 = A("l2_rc16", [128, 2, W], BF16)
            yacc = A("l2_yacc", [128, 2, W], F32)
            ras = [A(f"l2_ra{z}", [128, W], F32) for z in range(2)]
            ias = [A(f"l2_ia{z}", [128, W], F32) for z in range(2)]
            aas = [A(f"l2_aa{z}", [128, W], F32) for z in range(2)]
            s2s = [A(f"l2_s2{z}", [128, W], F32) for z in range(2)]
            hbs = [A("l2_hb", [128, W], F32)] * 2
            mo = [A(f"l2_mo{k}", [128, T], BF16) for k in range(2)]
            gw = [A(f"l2_gw{k}", [128, 4, 2, 256], BF16) for k in range(2)]
            pg = [PS(f"l2_pg{k}", [128, 512], F32) for k in range(5)]
            ptp = PS("l2_ptp", [128, 512], F32)
            self.dma('sp', prow[0][0:64, :], self.g('lru_conv_w')[j].rearrange("k (c p) -> (k c) p", p=128), r=[], w=['prow0'])
            self.dma('sp', prow[0][64:80, :], self.g('lru_conv_b')[j:j + 1, :].rearrange("o (c p) -> (o c) p", p=128), r=[], w=['prow0'])
            P.op('pool', lambda e: e.memset(prow[0][64:128, :], 0.0), r=[], w=['prow0z'])
            self.dma('sp', prow[1][0:64, :], self.g('lru_gate_b')[j].rearrange("d g (c p) -> (d g c) p", p=128), r=[], w=['prow1'])
            self.dma('sp', prow[1][64:96, :], self.g('lru_lambda')[j].rearrange("d (c p) -> (d c) p", p=128), r=[], w=['prow1'])
            P.op('pool', lambda e: e.memset(prow[1][64:128, :], 0.0), r=[], w=['prow1z'])
            P.barrier()
            self.dma('sp', prow[0][64:80, :], self.g('lru_conv_b')[j:j + 1, :].rearrange("o (c p) -> (o c) p", p=128), r=[], w=['prow0'])
            self.dma('sp', prow[1][64:96, :], self.g('lru_lambda')[j].rearrange("d (c p) -> (d c) p", p=128), r=[], w=['prow1'])
            for k in range(2):
                P.op('pe', lambda e, k=k: e.transpose(out=ptp[:, k * 128:(k + 1) * 128], in_=prow[k][:], identity=self.ident32[:]), r=[f'prow{k}', 'ident32'], w=['ptp'])
            P.op('act', lambda e: e.activation(out=pc_[:], in_=ptp[:, 0:256], func=AF.Copy), r=['ptp'], w=['pc'])
            P.op('act', lambda e: e.activation(out=cexp[:, 0:32], in_=pc_[:, 192:224], func=AF.Exp, scale=-1.0), r=['pc'], w=['cexp'])
            P.op('act', lambda e: e.activation(out=cexp[:, 0:32], in_=cexp[:, 0:32], func=AF.Ln, bias=1.0, scale=1.0), r=[], w=['cexp'])
            P.op('dve', lambda e: e.tensor_scalar(out=cexp[:, 32:64], in0=cexp[:, 0:32], scalar1=-16.0, scalar2=None, op0=ALU.mult), r=[], w=['cexp'])
            P.op('dve', lambda e: e.tensor_scalar(out=cexp[:, 0:32], in0=cexp[:, 0:32], scalar1=-8.0, scalar2=None, op0=ALU.mult), r=[], w=['cexp'])
            for k in range(2):
                P.op('pool', lambda e, k=k: e.memset(rp[k][:], 0.0), r=[], w=[f'rp{k}'])
            P.op('pool', lambda e: e.memset(yacc[:], 0.0), r=[], w=['yacc'])
            P.op('pool', lambda e: e.memset(hbs[0][:], 0.0), r=[], w=['hb'])
            itc = 0
            pieces = [(LAT0 + q * 512, min(512, CTX1 - (LAT0 + q * 512))) for q in range(5)]
            gwsrc = self.g('lru_gate_w')[j]
            for n in range(8):
                b2 = n % 2
                RP, UG, GW = rp[b2], ug[b2], gw[b2]
                for k in range(2):
                    row0 = 2048 + n * 256 + k * 128
                    self.dma('sp', RP[:, k, LAT0:LAT1], self.uT[row0:row0 + 128, 0:S], r=[], w=[f'rp{b2}'])
                    self.dma('sp', RP[:, k, CTX0:CTX1], self.uT[row0:row0 + 128, S:T], r=[], w=[f'rp{b2}'])
                    self.dma('act', UG[:, k, :], self.uT[n * 256 + k * 128:n * 256 + (k + 1) * 128, :], r=[], w=[f'ug_{k}'])
                for d_ in range(2):
                    for g_ in range(2):
                        P.op('pool', lambda e, GW=GW, d_=d_, g_=g_, n=n: e.dma_start(out=GW[:, d_ * 2 + g_, :, :], in_=gwsrc[d_, g_, n].rearrange("(kc p) o -> p kc o", p=128)), r=[], w=[f'gw{b2}'], dma=True)
                for k in range(2):
                    ch = n * 2 + k
                    P.op('act', lambda e, UG=UG, k=k: e.activation(out=UG[:, k, :], in_=UG[:, k, :], func=AF.Gelu_apprx_tanh), r=[], w=[f'ug_{k}'])
                    P.op('dve', lambda e, RP=RP, k=k, ch=ch: e.tensor_scalar(out=rc[:, k, LAT0:CTX1], in0=RP[:, k, 0:NW], scalar1=pc_[:, ch:ch + 1], scalar2=pc_[:, 64 + ch:65 + ch], op0=ALU.mult, op1=ALU.add), r=[f'rp{b2}', 'pc'], w=[f'rc{k}'])
                    for t_ in range(1, 4):
                        P.op('dve', lambda e, RP=RP, k=k, ch=ch, t_=t_: e.scalar_tensor_tensor(out=rc[:, k, LAT0:CTX1], in0=RP[:, k, t_:t_ + NW], scalar=pc_[:, t_ * 16 + ch:t_ * 16 + ch + 1], in1=rc[:, k, LAT0:CTX1], op0=ALU.mult, op1=ALU.add), r=[f'rp{b2}'], w=[f'rc{k}'])
                    P.op('act', lambda e, k=k: e.activation(out=rc16[:, k, LAT0:CTX1], in_=rc[:, k, LAT0:CTX1], func=AF.Copy), r=[f'rc{k}'], w=[f'rc16_{k}'])
                for d_ in range(2):
                    for jc in range(2):
                        ch = n * 2 + jc
                        z = itc % 2
                        itc += 1
                        ra, ia, aa, s2, hb = ras[z], ias[z], aas[z], s2s[z], hbs[z]
                        for g_, dstt, dres in ((0, ra, f'ra{z}'), (1, ia, f'ia{z}')):
                            bcol = pc_[:, 128 + (d_ * 2 + g_) * 16 + ch:128 + (d_ * 2 + g_) * 16 + ch + 1]
                            for pc, (p0, n_) in enumerate(pieces):
                                P.op('pe', lambda e, GW=GW, d_=d_, g_=g_, jc=jc, pc=pc, p0=p0, n_=n_: [e.matmul(pg[pc][:, 0:n_], lhsT=GW[:, d_ * 2 + g_, kc, jc * 128:(jc + 1) * 128], rhs=rc16[:, kc, p0:p0 + n_], start=(kc == 0), stop=(kc == 1)) for kc in range(2)],
                                     r=[f'gw{b2}', 'rc16_0', 'rc16_1'], w=[f'pg{pc}'])
                                P.op('act', lambda e, dstt=dstt, pc=pc, p0=p0, n_=n_, bcol=bcol: e.activation(out=dstt[:, p0:p0 + n_], in_=pg[pc][:, 0:n_], func=AF.Sigmoid, bias=bcol, scale=1.0), r=[f'pg{pc}', 'pc'], w=[dres])
                        c1 = cexp[:, d_ * 16 + ch:d_ * 16 + ch + 1]
                        c2 = cexp[:, 32 + d_ * 16 + ch:32 + d_ * 16 + ch + 1]
                        P.op('act', lambda e, c1=c1, aa=aa, ra=ra: e.activation(out=aa[:, LAT0:CTX1], in_=ra[:, LAT0:CTX1], func=AF.Exp, scale=c1), r=[f'ra{z}', 'cexp'], w=[f'aa{z}'])
                        P.op('act', lambda e, c2=c2, s2=s2, ra=ra: e.activation(out=s2[:, LAT0:CTX1], in_=ra[:, LAT0:CTX1], func=AF.Exp, scale=c2), r=[f'ra{z}', 'cexp'], w=[f's2{z}'])
                        P.op('act', lambda e, s2=s2: e.activation(out=s2[:, LAT0:CTX1], in_=s2[:, LAT0:CTX1], func=AF.Sqrt, scale=-1.0, bias=1.0), r=[], w=[f's2{z}'])
                        P.op('dve', lambda e, jc=jc, ia=ia: e.tensor_tensor(out=ia[:, LAT0:CTX1], in0=ia[:, LAT0:CTX1], in1=rc[:, jc, LAT0:CTX1], op=ALU.mult), r=[f'rc{jc}'], w=[f'ia{z}'])
                        P.op('dve', lambda e, ia=ia, s2=s2: e.tensor_tensor(out=ia[:, LAT0:CTX1], in0=ia[:, LAT0:CTX1], in1=s2[:, LAT0:CTX1], op=ALU.mult), r=[f's2{z}'], w=[f'ia{z}'])
                        if d_ == 0:
                            P.op('dve', lambda e, jc=jc, aa=aa, ia=ia: e.tensor_tensor_scan(out=yacc[:, jc, CTX0:CTX1], data0=aa[:, CTX0:CTX1], data1=ia[:, CTX0:CTX1], initial=0.0, op0=ALU.mult, op1=ALU.add), r=[f'aa{z}', f'ia{z}'], w=[f'yacc{jc}'])
                            P.op('dve', lambda e, jc=jc, aa=aa, ia=ia: e.tensor_tensor_scan(out=yacc[:, jc, LAT0:LAT1], data0=aa[:, LAT0:LAT1], data1=ia[:, LAT0:LAT1], initial=yacc[:, jc, CTX1 - 1:CTX1], op0=ALU.mult, op1=ALU.add), r=[f'aa{z}', f'ia{z}'], w=[f'yacc{jc}'])
                        else:
                            P.op('dve', lambda e, hb=hb, aa=aa, ia=ia: e.tensor_tensor_scan(out=rev_ap(hb[:, CTX0:CTX1]), data0=rev_ap(aa[:, CTX0:CTX1]), data1=rev_ap(ia[:, CTX0:CTX1]), initial=0.0, op0=ALU.mult, op1=ALU.add), r=[f'aa{z}', f'ia{z}'], w=['hb'])
                            P.op('dve', lambda e, hb=hb, aa=aa, ia=ia: e.tensor_tensor_scan(out=rev_ap(hb[:, LAT0:LAT1]), data0=rev_ap(aa[:, LAT0:LAT1]), data1=rev_ap(ia[:, LAT0:LAT1]), initial=hb[:, CTX0:CTX0 + 1], op0=ALU.mult, op1=ALU.add), r=[f'aa{z}', f'ia{z}'], w=['hb'])
                            P.op('dve', lambda e, jc=jc, hb=hb: e.tensor_tensor(out=yacc[:, jc, LAT0:CTX1], in0=yacc[:, jc, LAT0:CTX1], in1=hb[:, LAT0:CTX1], op=ALU.add), r=['hb'], w=[f'yacc{jc}'])
                for k in range(2):
                    MO = mo[k]
                    P.op('pool', lambda e, MO=MO, UG=UG, k=k: e.tensor_tensor(out=MO[:, 0:S], in0=yacc[:, k, LAT0:LAT1], in1=UG[:, k, 0:S], op=ALU.mult), r=[f'yacc{k}', f'ug_{k}'], w=[f'mo{k}'])
                    P.op('pool', lambda e, MO=MO, UG=UG, k=k: e.tensor_tensor(out=MO[:, S:T], in0=yacc[:, k, CTX0:CTX1], in1=UG[:, k, S:T], op=ALU.mult), r=[f'yacc{k}', f'ug_{k}'], w=[f'mo{k}'])
                    self.dma('sp', self.mT[(n * 2 + k) * 128:(n * 2 + k + 1) * 128, :], MO[:], r=[f'mo{k}'], w=[])
            P.barrier()


    def stage_att(self, j, i, srcname, need_ctx):
        nc, P = self.nc, self.P
        src = self.g('x_in') if srcname == 'x_in' else self.xs
        nq = NT if need_ctx else S // 128
        with ExitStack() as es0:
            A0 = lambda *a: es0.enter_context(nc.sbuf_tensor(self.nm(a[0]), *a[1:]))
            kTe = A0("at_kTe", [128, 4, T], BF16)
            kTo = A0("at_kTo", [128, 4, T], BF16)
            v1 = A0("at_v1", [128, NT, 4, 68], BF16)
            with ExitStack() as es:
                A = lambda *a: es.enter_context(nc.sbuf_tensor(self.nm(a[0]), *a[1:]))
                PS = lambda n, sh, dt: es.enter_context(nc.psum_tensor(self.nm(n), sh, dt))
                wq = A("t1_wq", [128, 16, 2560], BF16)
                sc1 = A("t1_sc", [128, D], F32)
                shb = A("t1_sh", [128, D], F32)
                xt = [A(f"t1_xt{k}", [128, D], F32) for k in range(2)]
                a16 = [A("t1_a16", [128, D], BF16)] * 2
                aTt = [A("t1_aT", [128, 16, 128], BF16)] * 2
                q32s = [A(f"t1_q32{z}", [128, D], F32) for z in range(2)]
                k32s = [A("t1_k32", [128, 256], F32)] * 2
                q16s = [A(f"t1_q16{z}", [128, D], BF16) for z in range(2)]
                k16s = [A(f"t1_k16{z}", [128, 256], BF16) for z in range(2)]
                kd = A("t1_kd", [128, 2, 4, 2, 64], BF16)
                tA = A("t1_tA", [128, 1024], F32)
                tB = A("t1_tB", [128, 1024], F32)
                qst = [A("t1_qst", [128, 16, 128], BF16)] * 2
                rts = [A(f"t1_rt{z}", [128, 64], F32) for z in range(2)]
                pTb = [PS(f"t1_pTb{k}", [128, 1024], BF16) for k in range(2)]
                po = [PS(f"t1_po{k}", [128, 512], F32) for k in range(5)]
                wsrc = self.g('attn_w_qkv')[j].rearrange("(c p) n -> p c n", p=128)
                for h in range(4):
                    for a_ in range(2):
                        P.op('pool', lambda e, h=h, a_=a_: e.dma_start(out=wq[:, 4 * h:4 * h + 4, a_ * 1280:(a_ + 1) * 1280], in_=wsrc[:, 4 * h:4 * h + 4, a_ * 1280:(a_ + 1) * 1280]), r=[], w=[f'wq{h}_{a_}'], dma=True)
                P.op('dve', lambda e: e.memset(v1[:].rearrange("p a b c -> p (a b c)"), 1.0), r=[], w=['v1'])
                P.op('dve', lambda e: e.memset(kd[:].rearrange("p a b c d -> p (a b c d)"), 0.0), r=[], w=['kd0', 'kd1'])
                for tt in range(NT):
                    v = 0 if tt < 16 else 1
                    k = tt % 2
                    if tt == 0 or tt == 16:
                        self.dma('sp', sc1[:], self.modb[i, 0, v, :, D:2 * D], r=[], w=['sc1'])
                        self.dma('act', shb[:], self.modb[i, 0, v, :, 0:D], r=[], w=['shb'])
                    X, A16, AT, QS = xt[k], a16[k], aTt[k], qst[k]
                    q32, k32, q16, k16 = q32s[k], k32s[k], q16s[k], k16s[k]
                    if tt == 0:
                        self.dma('sp', X[:], src[0:128, :], r=['xs0'], w=['x0'])
                    if tt + 1 < NT:
                        self.dma('sp', xt[(tt + 1) % 2][:], src[(tt + 1) * 128:(tt + 2) * 128, :], r=[f'xs{tt + 1}'], w=[f'x{(tt + 1) % 2}'])
                    P.op('dve', lambda e, X=X: e.tensor_tensor(out=X[:], in0=X[:], in1=sc1[:], op=ALU.mult), r=['sc1'], w=[f'x{k}'])
                    P.op('dve', lambda e, X=X: e.tensor_tensor(out=X[:], in0=X[:], in1=shb[:], op=ALU.add), r=['shb'], w=[f'x{k}'])
                    P.op('act', lambda e, X=X, A16=A16: e.activation(out=A16[:], in_=X[:], func=AF.Copy), r=[f'x{k}'], w=['a16s'])
                    for hh in range(2):
                        pt = pTb[hh]
                        P.op('pe', lambda e, pt=pt, A16=A16, hh=hh: [e.transpose(out=pt[:, jj * 128:(jj + 1) * 128], in_=A16[:, (8 * hh + jj) * 128:(8 * hh + jj + 1) * 128], identity=self.ident16[:]) for jj in range(8)],
                             r=['a16s', 'ident16'], w=[f'pTb{hh}'])
                        dst = AT[:, 8 * hh:8 * hh + 8, :].rearrange("p a b -> p (a b)")
                        if hh == 0:
                            P.op('act', lambda e, dst=dst, pt=pt: e.activation(out=dst, in_=pt[:], func=AF.Copy), r=[f'pTb{hh}'], w=[f'aTs_{hh}'])
                        else:
                            P.op('dve', lambda e, dst=dst, pt=pt: e.tensor_copy(out=dst, in_=pt[:]), r=[f'pTb{hh}'], w=[f'aTs_{hh}'])
                    need_q = tt < nq
                    for q in range(5):
                        if q < 4 and not need_q:
                            continue
                        P.op('pe', lambda e, AT=AT, q=q: [e.matmul(po[q][:], lhsT=AT[:, c, :], rhs=wq[:, c, q * 512:(q + 1) * 512], start=(c == 0), stop=(c == 15)) for c in range(16)],
                             r=['aTs_0', 'aTs_1'] + [f'wq{h}_{a_}' for h in range(4) for a_ in range(2)], w=[f'po{q}'])
                        if q < 4:
                            P.op('act', lambda e, q=q, q32=q32: e.activation(out=q32[:, q * 512:(q + 1) * 512], in_=po[q][:], func=AF.Copy, scale=0.125), r=[f'po{q}'], w=[f'q32{k}_{q}'])
                        else:
                            P.op('dve', lambda e, k32=k32: e.tensor_copy(out=k32[:], in_=po[4][:, 0:256]), r=['po4'], w=['k32'])
                            if 'skipD' not in self.dbg:
                                P.op('dve', lambda e, tt=tt: e.tensor_copy(out=v1[:, tt, :, 0:64], in_=po[4][:, 256:512].rearrange("p (g d) -> p g d", g=4)), r=['po4'], w=['v1'])
                    if tt < 16 and 'skipA' not in self.dbg:
                        rtk = rts[k]
                        self.dma('act', rtk[:], self.g('rope_t')[tt * 128:(tt + 1) * 128, :], r=[], w=[f'rt{k}'])

                        def rope(x32, x16, nh, xres, ores, tt=tt, rtk=rtk, k=k):
                            cos_ = sub_ap(rtk[:, 0:32], [(0, nh), (16, 2), (1, 16)])
                            sin_ = sub_ap(rtk[:, 32:64], [(0, nh), (16, 2), (1, 16)])
                            x1 = sub_ap(x32[:, 0:1], [(64, nh), (32, 2), (1, 16)])
                            x2 = sub_ap(x32[:, 16:17], [(64, nh), (32, 2), (1, 16)])
                            o1 = sub_ap(x16[:, 0:1], [(64, nh), (32, 2), (1, 16)])
                            o2 = sub_ap(x16[:, 16:17], [(64, nh), (32, 2), (1, 16)])
                            ta = sub_ap(tA[:, 0:1], [(32, nh), (16, 2), (1, 16)])
                            tb = sub_ap(tB[:, 0:1], [(32, nh), (16, 2), (1, 16)])
                            P.op('dve', lambda e: e.tensor_tensor(out=ta, in0=x1, in1=cos_, op=ALU.mult), r=xres + [f'rt{k}'], w=['tA'])
                            P.op('dve', lambda e: e.tensor_tensor(out=tb, in0=x2, in1=sin_, op=ALU.mult), r=xres + [f'rt{k}'], w=['tB'])
                            P.op('dve', lambda e: e.tensor_tensor(out=o1, in0=ta, in1=tb, op=ALU.subtract), r=['tA', 'tB'], w=[ores + 'a'])
                            P.op('dve', lambda e: e.tensor_tensor(out=ta, in0=x1, in1=sin_, op=ALU.mult), r=xres + [f'rt{k}'], w=['tA'])
                            P.op('dve', lambda e: e.tensor_tensor(out=tb, in0=x2, in1=cos_, op=ALU.mult), r=xres + [f'rt{k}'], w=['tB'])
                            P.op('dve', lambda e: e.tensor_tensor(out=o2, in0=ta, in1=tb, op=ALU.add), r=['tA', 'tB'], w=[ores + 'b'])
                        if need_q:
                            rope(q32, q16, 32, [f'q32{k}_{q}' for q in range(4)], f'q16{k}')
                        rope(k32, k16, 4, ['k32'], f'k16{k}')
                    elif tt >= 16:
                        if need_q:
                            P.op('act', lambda e, q16=q16, q32=q32: e.activation(out=q16[:], in_=q32[:], func=AF.Copy), r=[f'q32{k}_{q}' for q in range(4)], w=[f'q16{k}a', f'q16{k}b'])
                        P.op('dve', lambda e, k16=k16, k32=k32: e.tensor_copy(out=k16[:], in_=k32[:]), r=['k32'], w=[f'k16{k}a', f'k16{k}b'])
                    for dd in range(2 if 'skipB' not in self.dbg else 0):
                        P.op('dve', lambda e, dd=dd, k16=k16: e.tensor_copy(out=kd[:, dd, :, dd, :], in_=k16[:, :].rearrange("p (g d) -> p g d", g=4)), r=[f'k16{k}a', f'k16{k}b'], w=[f'kd{dd}'])
                    if 'skipB' not in self.dbg:
                        P.op('pe', lambda e: [e.transpose(out=pTb[0][:, (dd * 4 + g) * 128:(dd * 4 + g + 1) * 128], in_=kd[:, dd, g, :, :].rearrange("p a d -> p (a d)"), identity=self.ident16[:]) for dd in range(2) for g in range(4)],
                             r=['kd0', 'kd1', 'ident16'], w=['pTb0'])
                        P.op('dve', lambda e, tt=tt: e.tensor_copy(out=kTe[:, :, tt * 128:(tt + 1) * 128], in_=pTb[0][:, 0:512].rearrange("p (g t) -> p g t", g=4)), r=['pTb0'], w=['kTe'])
                        P.op('dve', lambda e, tt=tt: e.tensor_copy(out=kTo[:, :, tt * 128:(tt + 1) * 128], in_=pTb[0][:, 512:1024].rearrange("p (g t) -> p g t", g=4)), r=['pTb0'], w=['kTo'])
                    if need_q and 'skipC' not in self.dbg:
                        for hh in range(2):
                            pt = pTb[hh]
                            P.op('pe', lambda e, pt=pt, hh=hh, q16=q16: [e.transpose(out=pt[:, jj * 128:(jj + 1) * 128], in_=q16[:, (8 * hh + jj) * 128:(8 * hh + jj + 1) * 128], identity=self.ident16[:]) for jj in range(8)],
                                 r=[f'q16{k}a', f'q16{k}b', 'ident16'], w=[f'pTb{hh}'])
                            dst = QS[:, 8 * hh:8 * hh + 8, :].rearrange("p a b -> p (a b)")
                            if hh == 0:
                                P.op('act', lambda e, dst=dst, pt=pt: e.activation(out=dst, in_=pt[:], func=AF.Copy), r=[f'pTb{hh}'], w=['qst'])
                            else:
                                P.op('dve', lambda e, dst=dst, pt=pt: e.tensor_copy(out=dst, in_=pt[:]), r=[f'pTb{hh}'], w=['qst'])
                        self.dma('sp', self.qTd[tt], QS[:], r=['qst'], w=[])
                P.barrier()
            if 't1only' in self.dbg:
                self.dbg_k = self.scratch_out("dbg_kTd", [128, 4, T], BF16)
                kTd = kTe
                self.dbg_v = self.scratch_out("dbg_v1", [128, NT, 4, 68], BF16)
                self.dma('sp', self.dbg_k, kTd[:], r=[], w=[])
                self.dma('sp', self.dbg_v, v1[:], r=[], w=[])
                P.barrier()
                return
            with ExitStack() as es:
                A = lambda *a: es.enter_context(nc.sbuf_tensor(self.nm(a[0]), *a[1:]))
                PS = lambda n, sh, dt: es.enter_context(nc.psum_tensor(self.nm(n), sh, dt))
                qT = [A(f"t2_qT{k}", [128, 16, 128], BF16) for k in range(2)]
                PT = [[A(f"t2_PT{k}_{m}", [128, 1024], BF16) for m in range(5)] for k in range(2)]
                o16 = [A(f"t2_o16{k}", [128, D], BF16) for k in range(2)]
                oT = [A(f"t2_oT{k}", [128, 16, 128], BF16) for k in range(2)]
                srow = A("t2_srow", [1, 32], F32)
                esink = A("t2_esink", [128, 32], F32)
                m32 = A("t2_m32", [128, 2, 128], F32)
                m16 = A("t2_m16", [128, 2, 128], BF16)
                den = [A(f"t2_den{k}", [128, 16], F32) for k in range(2)]
                psS = [PS(f"t2_psS{k}", [128, 1024], F32) for k in range(2)]
                pso = PS("t2_pso", [128, 8, 128], F32)
                pTo = PS("t2_pTo", [128, 1024], BF16)
                self.dma('sp', srow[:], self.g('attn_sinks')[j:j + 1, :], r=[], w=['t2_row'])
                self.bcast_rows(None, (psS[0], 'psS0'), srow, esink, 't2')
                P.op('act', lambda e: e.activation(out=esink[:], in_=esink[:], func=AF.Exp), r=[], w=['t2'])
                self.dma('sp', m32[:], self.g('tri')[1:3].rearrange("m k q -> k m q"), r=[], w=['m32'])
                P.op('dve', lambda e: e.tensor_copy(out=m16[:], in_=m32[:]), r=['m32'], w=['m16'])
                ns = 0
                for qb in range(nq):
                    kq = qb % 2
                    QT = qT[kq]
                    self.dma('sp', QT[:], self.qTd[qb], r=[], w=[f'qT{kq}'])
                    if qb < 16:
                        kbs = [(kb, (1 if kb == qb - 1 else (2 if kb == qb + 1 else 0))) for kb in (qb - 1, qb, qb + 1) if 0 <= kb < 16] + [(16, 0), (17, 0)]
                    else:
                        kbs = [(16, 0), (17, 0)]
                    O16 = o16[kq]
                    for g in range(4):
                        PTs = PT[g % 2]
                        for m, (kb, mk) in enumerate(kbs):
                            pS = psS[ns % 2]
                            pSr = f'psS{ns % 2}'
                            ns += 1
                            P.op('pe', lambda e, pS=pS, g=g, kb=kb, QT=QT: [e.matmul(pS[:, (2 * cc + hh) * 128:(2 * cc + hh + 1) * 128], lhsT=(kTe if hh == 0 else kTo)[:, g, kb * 128:(kb + 1) * 128], rhs=QT[:, 4 * g + cc, :], start=True, stop=True) for cc in range(4) for hh in range(2)],
                                 r=[f'qT{kq}'], w=[pSr])
                            P.op('act', lambda e, pS=pS, PTs=PTs, m=m: [e.activation(out=PTs[m][:, 512 * z:512 * (z + 1)], in_=pS[:, 512 * z:512 * (z + 1)], func=AF.Exp) for z in range(2)], r=[pSr], w=[f'PT{g % 2}_{m}'])
                            if mk and 'skipE' not in self.dbg:
                                P.op('dve', lambda e, PTs=PTs, m=m, mk=mk: e.tensor_tensor(out=PTs[m][:, :].rearrange("p (h q) -> p h q", h=8), in0=PTs[m][:, :].rearrange("p (h q) -> p h q", h=8), in1=sub_ap(m16[:, mk - 1, :], [(0, 8), (1, 128)]), op=ALU.mult), r=['m16'], w=[f'PT{g % 2}_{m}'])
                        nk = len(kbs)
                        if 'skipF' in self.dbg:
                            continue
                        P.op('pe', lambda e, PTs=PTs, kbs=kbs, g=g, nk=nk: [e.matmul(pso[:, hl, 0:65], lhsT=PTs[m][:, hl * 128:(hl + 1) * 128], rhs=v1[:, kb, g, 0:65], start=(m == 0), stop=(m == nk - 1)) for hl in range(8) for m, (kb, mk) in enumerate(kbs)],
                             r=[f'PT{g % 2}_{m}' for m in range(nk)], w=['pso'])
                        DN = den[g % 2]
                        P.op('dve', lambda e, DN=DN, g=g: e.tensor_tensor(out=DN[:, 0:8], in0=pso[:, :, 64], in1=esink[:, 8 * g:8 * g + 8], op=ALU.add), r=['pso', 't2'], w=[f'den{g % 2}'])
                        P.op('dve', lambda e, DN=DN: e.reciprocal(out=DN[:, 8:16], in_=DN[:, 0:8]), r=[], w=[f'den{g % 2}'])
                        P.op('dve', lambda e, DN=DN, O16=O16, g=g: e.tensor_tensor(out=O16[:, 512 * g:512 * (g + 1)].rearrange("p (h d) -> p h d", h=8), in0=pso[:, :, 0:64], in1=sub_ap(DN[:, 8:16], [(1, 8), (0, 64)]), op=ALU.mult), r=['pso', f'den{g % 2}'], w=[f'o16{kq}_{g}'])
                    if 'skipF' in self.dbg or 'skipG' in self.dbg:
                        continue
                    OT = oT[kq]
                    for hh in range(2):
                        P.op('pe', lambda e, O16=O16, hh=hh: [e.transpose(out=pTo[:, jj * 128:(jj + 1) * 128], in_=O16[:, (8 * hh + jj) * 128:(8 * hh + jj + 1) * 128], identity=self.ident16[:]) for jj in range(8)],
                             r=[f'o16{kq}_{g}' for g in range(4)] + ['ident16'], w=['t2pTb'])
                        dst = OT[:, 8 * hh:8 * hh + 8, :].rearrange("p a b -> p (a b)")
                        P.op('act', lambda e, dst=dst: e.activation(out=dst, in_=pTo[:], func=AF.Copy), r=['t2pTb'], w=[f'oT{kq}'])
                    self.dma('sp', self.mT.rearrange("(c p) t -> p c t", p=128)[:, :, qb * 128:(qb + 1) * 128], OT[:], r=[f'oT{kq}'], w=[])
                P.barrier()


def build(plan, dbg=()):
    B = Builder(plan, dbg)
    B.declare_io()
    B.consts()
    for st in plan:
        getattr(B, 'stage_' + st[0])(*st[1:])
    B.P.finish()
    return B


def host_consts():
    pos = np.arange(S)
    rowp = (pos // 64).astype(np.float32)
    colp = (pos % 64).astype(np.float32)
    freqs = (10000.0 ** (-np.arange(0, 32, 2, dtype=np.float32) / 32)).astype(np.float32)
    ar = rowp[:, None] * freqs[None, :]
    ac = colp[:, None] * freqs[None, :]
    rope = np.concatenate([np.cos(ar), np.cos(ac), np.sin(ar), np.sin(ac)], axis=1).astype(np.float32)
    kk = np.arange(128)[:, None]
    qq = np.arange(128)[None, :]
    tri = np.stack([(kk < qq), (qq <= kk), (kk <= qq)]).astype(np.float32)
    iota = np.tile(np.arange(512, dtype=np.float32)[None, :], (128, 1))
    return {"ident": np.eye(128, dtype=np.float32), "rope_t": rope, "tri": tri, "iota": iota,
            "pcol": np.arange(128, dtype=np.float32).reshape(128, 1)}


WNAMES = ['ada_w', 'ada_b', 'ln_g', 'ln_b', 'lru_w_in', 'lru_conv_w', 'lru_conv_b', 'lru_gate_w',
          'lru_gate_b', 'lru_lambda', 'lru_w_out', 'attn_w_qkv', 'attn_sinks', 'attn_w_o', 'router_w',
          'router_b', 'moe_w_gu', 'moe_b_gu', 'moe_w_down', 'moe_b_down']


def make_in_maps(inputs, cores, x_override=None):
    hc = host_consts()
    maps = []
    for b in cores:
        m = {k: np.ascontiguousarray(inputs[k]) for k in WNAMES if k in inputs}
        m.update(hc)
        if x_override is not None:
            m["x_in"] = x_override[b]
        else:
            m["x_in"] = np.ascontiguousarray(np.concatenate([inputs['x'][b], inputs['ctx'][b]], axis=0))
        m["cvec"] = np.ascontiguousarray(np.stack([inputs['c'][b], inputs['c_ctx']]).reshape(32, 128))
        maps.append(m)
    return maps


def full_plan():
    plan = [('ada', [0, 1, 2, 3])]
    for i in range(DEPTH):
        need_ctx = i < DEPTH - 1
        srcname = 'x_in' if i == 0 else 'xs'
        j = i // 2
        if i % 2 == 0:
            plan.append(('lru', j, i, srcname))
            plan.append(('proj', i, 'lru_w_out', j, srcname, True))
        else:
            plan.append(('att', j, i, srcname, need_ctx))
            plan.append(('proj', i, 'attn_w_o', j, srcname, need_ctx))
        plan.append(('moe', i, 'xs', 'y_out' if i == DEPTH - 1 else 'xs', need_ctx))
    return plan


_CACHE = {}


def kernel(**inputs):
    inputs = {k: np.asarray(v) for k, v in inputs.items()}
    nb = inputs['x'].shape[0]
    if 'B' not in _CACHE:
        _CACHE['B'] = build(full_plan())
    B = _CACHE['B']
    need = list(B.din.keys())
    maps = make_in_maps(inputs, list(range(nb)))
    maps = [{k: np.ascontiguousarray(m[k], dtype=np.float32) for k in need} for m in maps]
    res = run_bass_kernel_spmd(B.nc, maps, core_ids=list(range(nb)))
    out = np.stack([np.asarray(r['y_out'], dtype=np.float32) for r in res.results], axis=0)
    return out
```
